# Optimizing a Trainium2 kernel written in Bass

```python
import jax
import jax.numpy as jnp
from jax import lax
import numpy as np

D_MODEL = 2048
BATCH = 1
SEQ = 8192
DEPTH = 4

GRID_W = 64
CTX_LEN = 256
NORM_EPS = 1e-6

A_HEADS = 8
A_KDIM = 128
A_VDIM = 128
A_KEY = A_HEADS * A_KDIM
A_WIDTH = A_HEADS * A_VDIM
A_CHUNK = 64

B_WIDTH = 1024
B_KSIZE = 31

C_QHEADS = 8
C_KVHEADS = 2
C_HDIM = 128
C_WIDTH = C_QHEADS * C_HDIM
C_KVWIDTH = C_KVHEADS * C_HDIM
C_WINDOW = 128
C_BLOCK = 128
ROPE_BASE = 10000.0

N_BRANCH = 3

N_EXPERTS = 16
EC_CAPACITY = 2
EXPERT_DFF = 1024

IN_SIZES = (A_KEY, A_WIDTH, A_KEY, A_KEY, A_WIDTH, 2 * B_WIDTH, C_WIDTH, C_KVWIDTH, C_KVWIDTH, N_BRANCH * D_MODEL)

kernel_name = 'hybrid_hgrn2_conv_swa_ecmoe_dit'


def rms_norm(x, g):
    xf = x.astype(jnp.float32)
    y = xf * lax.rsqrt(jnp.mean(xf * xf, axis=-1, keepdims=True) + NORM_EPS)
    return (y * g.astype(jnp.float32)).astype(x.dtype)


def layer_norm(x, g, b):
    xf = x.astype(jnp.float32)
    xc = xf - jnp.mean(xf, axis=-1, keepdims=True)
    y = xc * lax.rsqrt(jnp.mean(xc * xc, axis=-1, keepdims=True) + NORM_EPS)
    return (y * g.astype(jnp.float32) + b.astype(jnp.float32)).astype(x.dtype)


def split_in(p):
    offsets = np.cumsum(IN_SIZES)[:-1].tolist()
    return jnp.split(p, offsets, axis=-1)


def flip_seq(t):
    return jnp.flip(t, axis=1)


def hgrn_lower_bounds(lb_logits):
    p = jax.nn.softmax(lb_logits.astype(jnp.float32), axis=0)
    cs = jnp.cumsum(p, axis=0)
    return cs - cs[0]


def hgrn_forget(z, lb):
    zf = z.astype(jnp.float32)
    log_f = jnp.logaddexp(jnp.log(lb), jnp.log1p(-lb) + jax.nn.log_sigmoid(zf))
    k = (1.0 - lb) * jax.nn.sigmoid(-zf)
    return log_f, k


def hgrn_chunk_scan(q, k, v, log_f, s0):
    bsz, seq_len, heads, _ = q.shape
    vdim = v.shape[-1]
    n_chunks = seq_len // A_CHUNK

    def chunks(t):
        return t.astype(jnp.float32).reshape(bsz, n_chunks, A_CHUNK, heads, t.shape[-1]).transpose(1, 0, 3, 2, 4)

    tri = jnp.tril(jnp.ones((A_CHUNK, A_CHUNK), dtype=bool))

    def step(s, inp):
        qc, kc, vc, gc = inp
        b = jnp.cumsum(gc, axis=2)
        rel = jnp.where(tri[:, :, None], b[:, :, :, None, :] - b[:, :, None, :, :], -jnp.inf)
        attn = jnp.einsum('bhtk,bhtsk,bhsk->bhts', qc, jnp.exp(rel), kc)
        o = jnp.einsum('bhts,bhsv->bhtv', attn, vc) + jnp.einsum('bhtk,bhkv->bhtv', qc * jnp.exp(b), s)
        b_end = b[:, :, -1, :]
        s_new = jnp.exp(b_end)[..., None] * s + jnp.einsum('bhsk,bhsv->bhkv', kc * jnp.exp(b_end[:, :, None, :] - b), vc)
        return s_new, o

    s_fin, o = lax.scan(step, s0, (chunks(q), chunks(k), chunks(v), chunks(log_f)))
    o = o.transpose(1, 0, 3, 2, 4).reshape(bsz, seq_len, heads, vdim)
    return o, s_fin


def hgrn_mixer(p_lat, p_ctx, lb_fwd, lb_bwd, onorm_g, with_ctx):
    def heads(t):
        return t.reshape(t.shape[0], t.shape[1], A_HEADS, -1)

    def prep(p):
        q, v, zf, zb, g = p
        lf_f, k_f = hgrn_forget(zf, lb_fwd)
        lf_b, k_b = hgrn_forget(zb, lb_bwd)
        return heads(q), heads(v), heads(lf_f), heads(k_f), heads(lf_b), heads(k_b), g

    def bidir(t, s0_f, s0_b):
        q, v, lf_f, k_f, lf_b, k_b, _ = t
        o_f, s_f = hgrn_chunk_scan(q, k_f, v, lf_f, s0_f)
        o_b, s_b = hgrn_chunk_scan(flip_seq(q), flip_seq(k_b), flip_seq(v), flip_seq(lf_b), s0_b)
        return o_f + flip_seq(o_b), s_f, s_b

    def readout(o, g):
        on = rms_norm(o, onorm_g.reshape(A_HEADS, A_VDIM))
        on = on.reshape(o.shape[0], o.shape[1], A_WIDTH)
        return (on * jax.nn.silu(g.astype(jnp.float32))).astype(g.dtype)

    t_ctx = prep(p_ctx)
    t_lat = prep(p_lat)
    s0 = jnp.zeros((p_lat[0].shape[0], A_HEADS, A_KDIM, A_VDIM), jnp.float32)
    o_ctx, s_f, s_b = bidir(t_ctx, s0, s0)
    o_lat, _, _ = bidir(t_lat, s_f, s_b)
    y_lat = readout(o_lat, t_lat[-1])
    y_ctx = readout(o_ctx, t_ctx[-1]) if with_ctx else None
    return y_lat, y_ctx


def conv_module(u, conv_w, conv_b, ln_g, ln_b):
    a, gate = jnp.split(u, 2, axis=-1)
    h = a * jax.nn.sigmoid(gate)
    h = lax.conv_general_dilated(h, conv_w[:, None, :], window_strides=(1,),
                                 padding=[(B_KSIZE // 2, B_KSIZE // 2)],
                                 dimension_numbers=('NWC', 'WIO', 'NWC'),
                                 feature_group_count=B_WIDTH) + conv_b
    return jax.nn.silu(layer_norm(h, ln_g, ln_b))


def axial_rope_tables(s_len):
    n_rows = s_len // GRID_W
    rows = jnp.broadcast_to(jnp.arange(n_rows, dtype=jnp.float32)[:, None], (n_rows, GRID_W)).reshape(-1)
    cols = jnp.broadcast_to(jnp.arange(GRID_W, dtype=jnp.float32)[None, :], (n_rows, GRID_W)).reshape(-1)
    half = C_HDIM // 2
    inv = ROPE_BASE ** (-jnp.arange(0, half, 2, dtype=jnp.float32) / half)
    ang_r = rows[:, None] * inv
    ang_c = cols[:, None] * inv
    return jnp.cos(ang_r), jnp.sin(ang_r), jnp.cos(ang_c), jnp.sin(ang_c)


def apply_axial_rope(x, tables):
    cos_r, sin_r, cos_c, sin_c = tables
    half = C_HDIM // 2

    def rot(xh, cos, sin):
        x1, x2 = jnp.split(xh.astype(jnp.float32), 2, axis=-1)
        cos = cos[None, :, None, :]
        sin = sin[None, :, None, :]
        return jnp.concatenate([x1 * cos - x2 * sin, x1 * sin + x2 * cos], axis=-1)

    out = jnp.concatenate([rot(x[..., :half], cos_r, sin_r), rot(x[..., half:], cos_c, sin_c)], axis=-1)
    return out.astype(x.dtype)


def softmax_with_sink(logits, sink):
    sink_col = jnp.broadcast_to(sink, logits.shape[:-1] + (1,))
    p = jax.nn.softmax(jnp.concatenate([logits, sink_col], axis=-1), axis=-1)
    return p[..., :-1]


def window_attention(p_lat, p_ctx, rope, sink, with_ctx):
    q, k, v = p_lat
    cq, ck, cv = p_ctx
    bsz, s_len = q.shape[0], q.shape[1]
    n_ctx = ck.shape[1]
    grp = C_QHEADS // C_KVHEADS
    nb = s_len // C_BLOCK
    scale = C_HDIM ** -0.5
    sink_g = sink.astype(jnp.float32).reshape(C_KVHEADS, grp)

    q = apply_axial_rope(q.reshape(bsz, s_len, C_QHEADS, C_HDIM), rope)
    k = apply_axial_rope(k.reshape(bsz, s_len, C_KVHEADS, C_HDIM), rope)
    v = v.reshape(bsz, s_len, C_KVHEADS, C_HDIM)
    ck = ck.reshape(bsz, n_ctx, C_KVHEADS, C_HDIM)
    cv = cv.reshape(bsz, n_ctx, C_KVHEADS, C_HDIM)

    qb = q.reshape(bsz, nb, C_BLOCK, C_KVHEADS, grp, C_HDIM)
    pad = ((0, 0), (C_BLOCK, C_BLOCK), (0, 0), (0, 0))
    kp = jnp.pad(k, pad).reshape(bsz, nb + 2, C_BLOCK, C_KVHEADS, C_HDIM)
    vp = jnp.pad(v, pad).reshape(bsz, nb + 2, C_BLOCK, C_KVHEADS, C_HDIM)
    kn = jnp.concatenate([kp[:, 0:nb], kp[:, 1:nb + 1], kp[:, 2:nb + 2]], axis=2)
    vn = jnp.concatenate([vp[:, 0:nb], vp[:, 1:nb + 1], vp[:, 2:nb + 2]], axis=2)

    qpos = jnp.arange(nb)[:, None] * C_BLOCK + jnp.arange(C_BLOCK)[None, :]
    kpos = (jnp.arange(nb)[:, None] - 1) * C_BLOCK + jnp.arange(3 * C_BLOCK)[None, :]
    valid = ((jnp.abs(qpos[:, :, None] - kpos[:, None, :]) <= C_WINDOW)
             & (kpos[:, None, :] >= 0) & (kpos[:, None, :] < s_len))

    s_loc = jnp.einsum('bnqhgd,bnkhd->bnhgqk', qb, kn).astype(jnp.float32) * scale
    s_loc = jnp.where(valid[None, :, None, None], s_loc, -jnp.inf)
    s_ctx = jnp.einsum('bnqhgd,bkhd->bnhgqk', qb, ck).astype(jnp.float32) * scale
    p = softmax_with_sink(jnp.concatenate([s_loc, s_ctx], axis=-1), sink_g[None, None, :, :, None, None])
    p_loc = p[..., :3 * C_BLOCK].astype(v.dtype)
    p_ctx = p[..., 3 * C_BLOCK:].astype(v.dtype)
    o = jnp.einsum('bnhgqk,bnkhd->bnqhgd', p_loc, vn) + jnp.einsum('bnhgqk,bkhd->bnqhgd', p_ctx, cv)
    y_lat = o.reshape(bsz, s_len, C_WIDTH)

    y_ctx = None
    if with_ctx:
        cqg = cq.reshape(bsz, n_ctx, C_KVHEADS, grp, C_HDIM)
        sc = jnp.einsum('bqhgd,bkhd->bhgqk', cqg, ck).astype(jnp.float32) * scale
        pc = softmax_with_sink(sc, sink_g[None, :, :, None, None]).astype(cv.dtype)
        y_ctx = jnp.einsum('bhgqk,bkhd->bqhgd', pc, cv).reshape(bsz, n_ctx, C_WIDTH)
    return y_lat, y_ctx


def token_mixer(hx, hc, rope, lb_fwd, lb_bwd, w_in, onorm_g, conv_w, conv_b, ln_g, ln_b, sink,
                w_a, w_b, w_c, w_out, with_ctx):
    px = split_in(hx @ w_in)
    pc = split_in(hc @ w_in)
    ya_x, ya_c = hgrn_mixer(px[0:5], pc[0:5], lb_fwd, lb_bwd, onorm_g, with_ctx)
    yc_x, yc_c = window_attention(px[6:9], pc[6:9], rope, sink, with_ctx)
    yb_x = conv_module(px[5], conv_w, conv_b, ln_g, ln_b)

    def merge(ya, yb, yc, gate_logits):
        ga, gb, gc = jnp.split(jax.nn.sigmoid(gate_logits), N_BRANCH, axis=-1)
        return (ga * (ya @ w_a) + gb * (yb @ w_b) + gc * (yc @ w_c)) @ w_out

    y_x = merge(ya_x, yb_x, yc_x, px[9])
    y_c = None
    if with_ctx:
        yb_c = conv_module(pc[5], conv_w, conv_b, ln_g, ln_b)
        y_c = merge(ya_c, yb_c, yc_c, pc[9])
    return y_x, y_c


def expert_choice_moe(h, w_router, w_gate, w_up, w_down):
    bsz, n_tok, dim = h.shape
    cap = EC_CAPACITY * n_tok // N_EXPERTS
    aff = jax.nn.softmax((h @ w_router).astype(jnp.float32), axis=-1)
    gate, idx = lax.top_k(jnp.swapaxes(aff, 1, 2), cap)
    xs = jax.vmap(lambda hb, ib: hb[ib])(h, idx)
    hid = jax.nn.silu(jnp.einsum('becd,edf->becf', xs, w_gate)) * jnp.einsum('becd,edf->becf', xs, w_up)
    ys = jnp.einsum('becf,efd->becd', hid, w_down) * gate[..., None].astype(h.dtype)
    return jax.vmap(lambda ib, yb: jnp.zeros((n_tok, dim), yb.dtype).at[ib.reshape(-1)].add(yb.reshape(-1, dim)))(idx, ys)


def setup_inputs(seed: int = 0) -> dict:
    key = jax.random.key(seed)
    ks = jax.random.split(key, 32)
    f32 = jnp.float32
    dm = D_MODEL
    d_in = sum(IN_SIZES)

    def nrm(k, shape, scale):
        return jax.random.normal(k, shape, f32) * scale

    return {
        'x': nrm(ks[0], (BATCH, SEQ, dm), 1.0),
        'c': nrm(ks[1], (BATCH, dm), 1.0),
        'ctx': nrm(ks[2], (BATCH, CTX_LEN, dm), 1.0),
        'c_ctx': nrm(ks[3], (dm,), 1.0),
        'w_ada': nrm(ks[4], (DEPTH, dm, 6 * dm), 0.5 * dm ** -0.5),
        'b_ada': nrm(ks[5], (DEPTH, 6 * dm), 0.02),
        'norm1_g': 1.0 + nrm(ks[6], (DEPTH, dm), 0.02),
        'norm2_g': 1.0 + nrm(ks[7], (DEPTH, dm), 0.02),
        'w_in': nrm(ks[8], (DEPTH, dm, d_in), dm ** -0.5),
        'hgrn_lb_logits': nrm(ks[9], (DEPTH, 2, A_KEY), 0.5),
        'hgrn_onorm_g': 1.0 + nrm(ks[10], (DEPTH, A_WIDTH), 0.02),
        'conv_w': nrm(ks[11], (DEPTH, B_KSIZE, B_WIDTH), B_KSIZE ** -0.5),
        'conv_b': nrm(ks[12], (DEPTH, B_WIDTH), 0.02),
        'conv_ln_g': 1.0 + nrm(ks[13], (DEPTH, B_WIDTH), 0.02),
        'conv_ln_b': nrm(ks[14], (DEPTH, B_WIDTH), 0.02),
        'attn_sink': nrm(ks[15], (DEPTH, C_QHEADS), 0.5),
        'w_branch_a': nrm(ks[16], (DEPTH, A_WIDTH, dm), A_WIDTH ** -0.5),
        'w_branch_b': nrm(ks[17], (DEPTH, B_WIDTH, dm), B_WIDTH ** -0.5),
        'w_branch_c': nrm(ks[18], (DEPTH, C_WIDTH, dm), C_WIDTH ** -0.5),
        'w_out': nrm(ks[19], (DEPTH, dm, dm), dm ** -0.5),
        'w_router': nrm(ks[20], (DEPTH, dm, N_EXPERTS), dm ** -0.5),
        'w_exp_gate': nrm(ks[21], (DEPTH, N_EXPERTS, dm, EXPERT_DFF), dm ** -0.5),
        'w_exp_up': nrm(ks[22], (DEPTH, N_EXPERTS, dm, EXPERT_DFF), dm ** -0.5),
        'w_exp_down': nrm(ks[23], (DEPTH, N_EXPERTS, EXPERT_DFF, dm), EXPERT_DFF ** -0.5),
        'final_norm_g': 1.0 + nrm(ks[24], (dm,), 0.02),
    }


def reference(x, c, ctx, c_ctx, w_ada, b_ada, norm1_g, norm2_g, w_in, hgrn_lb_logits, hgrn_onorm_g,
              conv_w, conv_b, conv_ln_g, conv_ln_b, attn_sink, w_branch_a, w_branch_b, w_branch_c,
              w_out, w_router, w_exp_gate, w_exp_up, w_exp_down, final_norm_g):
    rope = axial_rope_tables(x.shape[1])
    lbs = hgrn_lower_bounds(hgrn_lb_logits)
    c_lat = jax.nn.silu(c)
    c_con = jax.nn.silu(c_ctx)
    cx = ctx
    for l in range(DEPTH):
        with_ctx = l < DEPTH - 1
        mod_x = jnp.split((c_lat @ w_ada[l] + b_ada[l])[:, None, :], 6, axis=-1)
        mod_c = jnp.split(c_con @ w_ada[l] + b_ada[l], 6, axis=-1)
        hx = rms_norm(x, norm1_g[l]) * (1.0 + mod_x[1]) + mod_x[0]
        hc = rms_norm(cx, norm1_g[l]) * (1.0 + mod_c[1]) + mod_c[0]
        y_x, y_c = token_mixer(hx, hc, rope, lbs[l, 0], lbs[l, 1], w_in[l], hgrn_onorm_g[l],
                               conv_w[l], conv_b[l], conv_ln_g[l], conv_ln_b[l], attn_sink[l],
                               w_branch_a[l], w_branch_b[l], w_branch_c[l], w_out[l], with_ctx)
        x = x + mod_x[2] * y_x
        hx = rms_norm(x, norm2_g[l]) * (1.0 + mod_x[4]) + mod_x[3]
        x = x + mod_x[5] * expert_choice_moe(hx, w_router[l], w_exp_gate[l], w_exp_up[l], w_exp_down[l])
        if with_ctx:
            cx = cx + mod_c[2] * y_c
            hc = rms_norm(cx, norm2_g[l]) * (1.0 + mod_c[4]) + mod_c[3]
            cx = cx + mod_c[5] * expert_choice_moe(hc, w_router[l], w_exp_gate[l], w_exp_up[l], w_exp_down[l])
    return rms_norm(x, final_norm_g)
```

```python
import numpy as np
import concourse.bass as bass
import concourse.mybir as mybir
from concourse.bass_utils import run_bass_kernel_spmd
from contextlib import ExitStack

F32 = mybir.dt.float32
BF16 = mybir.dt.bfloat16
I32 = mybir.dt.int32
AF = mybir.ActivationFunctionType
ALU = mybir.AluOpType
AX = mybir.AxisListType

D = 2048
KD = 16
TC = 256
TL = 8192
T = TC + TL
DEPTH = 4
EPS = 1e-6
NE = 16
DFF = 1024
CAPL = 1024
CAPC = 32
SLOTS = CAPL + CAPC
SROWS = SLOTS + 1
BLOCKS = [(0, TC)] + [(TC + 512 * i, 512) for i in range(TL // 512)]
DIN = 14848


class K:
    ROT = 60000

    def __init__(self, nc, n_dma_sems=48):
        self.nc = nc
        self.es = ExitStack()
        self.engs = {"pe": nc.tensor, "dve": nc.vector, "act": nc.scalar, "pool": nc.gpsimd, "sp": nc.sync}
        self.sem = {}
        self.cnt = {}
        self.nsem = 0
        for e in self.engs:
            self.sem[e] = self._newsem(e)
            self.cnt[e] = 0
        self.dma_sems = [self._newsem("dma%d" % i) for i in range(n_dma_sems)]
        self.dma_val = [0] * n_dma_sems
        self.dma_rr = 0
        self.seen = {e: {} for e in self.engs}
        self.lastw = {}
        self.readers = {}
        self.ninst = 0
        self.qrr = 0

    def _newsem(self, name):
        self.nsem += 1
        return self.es.enter_context(self.nc.semaphore("s_%s_%d" % (name, self.nsem)))

    def _wait(self, e, dep):
        s, v = dep
        sid = id(s)
        if self.seen[e].get(sid, 0) >= v:
            return
        self.engs[e].wait_ge(s, v)
        self.seen[e][sid] = v

    def _deps(self, e, reads, writes):
        for k in reads:
            d = self.lastw.get(k)
            if d is not None and not (e == "pe" and d[2] == "pe"):
                self._wait(e, (d[0], d[1]))
        for k in writes:
            d = self.lastw.get(k)
            if d is not None and not (e == "pe" and d[2] == "pe"):
                self._wait(e, (d[0], d[1]))
            for d in self.readers.get(k, ()):
                if not (e == "pe" and d[2] == "pe"):
                    self._wait(e, (d[0], d[1]))

    def _record(self, dep, reads, writes):
        for k in writes:
            self.lastw[k] = dep
            self.readers[k] = []
        for k in reads:
            self.readers.setdefault(k, []).append(dep)

    def op(self, e, fn, reads=(), writes=()):
        self._deps(e, reads, writes)
        inst = fn()
        if self.cnt[e] >= self.ROT:
            self.sem[e] = self._newsem(e)
            self.cnt[e] = 0
        self.cnt[e] += 1
        inst.then_inc(self.sem[e], 1)
        self._record((self.sem[e], self.cnt[e], e), reads, writes)
        self.ninst += 1
        return inst

    def dma(self, q, out, in_, reads=(), writes=(), indirect=None, **kw):
        if q is None:
            q = ("sp", "act")[self.qrr % 2]
            self.qrr += 1
        self._deps(q, reads, writes)
        i = self.dma_rr
        self.dma_rr = (self.dma_rr + 1) % len(self.dma_sems)
        s = self.dma_sems[i]
        if self.dma_val[i] > 0:
            self._wait(q, (s, self.dma_val[i]))
        if indirect is None:
            inst = self.engs[q].dma_start(out=out, in_=in_, **kw)
        else:
            inst = self.engs[q].indirect_dma_start(out=out, in_=in_, **indirect)
        self.dma_val[i] += 16
        inst.then_inc(s, 16)
        self._record((s, self.dma_val[i], "dma"), reads, writes)
        self.ninst += 1
        return inst

    def finish(self, keys):
        for k in keys:
            d = self.lastw.get(k)
            if d is not None:
                self._wait("sp", (d[0], d[1]))

    def sb(self, st, name, shape, dt):
        self.uid = getattr(self, "uid", 0) + 1
        return st.enter_context(self.nc.sbuf_tensor("%s_%d" % (name, self.uid), list(shape), dt))

    def ps(self, st, name, shape, dt=F32):
        self.uid = getattr(self, "uid", 0) + 1
        return st.enter_context(self.nc.psum_tensor("%s_%d" % (name, self.uid), list(shape), dt))


class Ctx:
    pass


def fm(ap, c0, w):
    return ap.rearrange("(k p) t -> p k t", p=128)[:, :, c0:c0 + w]


def phase_mods(g, layers):
    k, nc = g.k, g.nc
    with ExitStack() as st:
        cc = k.sb(st, "cc", [128, KD, 2], F32)
        sc = k.sb(st, "sc", [128, KD, 2], F32)
        bada = k.sb(st, "bada", [128, DEPTH, 6, KD], F32)
        wst = [k.sb(st, "wada%d" % i, [128, KD, 512], F32) for i in range(2)]
        ps = k.ps(st, "psm", [128, 512])
        k.dma("sp", cc[:], g.cvec, writes=["cc"])
        k.dma("act", bada[:], g.b_ada, writes=["bada"])
        k.op("act", lambda: nc.scalar.activation(out=sc[:], in_=cc[:], func=AF.Silu), reads=["cc"], writes=["sc"])
        it = 0
        for l in layers:
            for j in range(6):
                for q in range(4):
                    w = wst[it % 2]
                    wk = ("wada", it % 2)
                    col0 = j * D + q * 512
                    k.dma(None, w[:], g.w_ada[l].rearrange("(k p) c -> p k c", p=128)[:, :, col0:col0 + 512],
                          writes=[wk])
                    for m in range(4):
                        for kk in range(KD):
                            k.op("pe", lambda: nc.tensor.matmul(ps[:, m * 2:m * 2 + 2], lhsT=w[:, kk, m * 128:(m + 1) * 128],
                                                                 rhs=sc[:, kk, :], start=(kk == 0), stop=(kk == KD - 1)),
                                 reads=[wk, "sc"], writes=["psm"])
                    for m in range(4):
                        kc = q * 4 + m
                        k.op("dve", lambda: nc.vector.tensor_scalar(out=g.mods[:, l, j, kc, :], in0=ps[:, m * 2:m * 2 + 2],
                                                                    scalar1=bada[:, l, j, kc:kc + 1], scalar2=None, op0=ALU.add),
                             reads=["psm", "bada"], writes=["mods"])
                    it += 1
        for l in layers:
            for (dst, gsrc, j) in ((g.gs1, g.n1g, 1), (g.gs2, g.n2g, 4)):
                k.op("dve", lambda: nc.vector.tensor_scalar(out=dst[:, l, :, :], in0=g.mods[:, l, j, :, :], scalar1=1.0, scalar2=None, op0=ALU.add),
                     reads=["mods"], writes=["gs"])
                k.op("dve", lambda: nc.vector.tensor_tensor(out=dst[:, l, :, :], in0=dst[:, l, :, :],
                                                            in1=gsrc[:, l, :].unsqueeze(2).to_broadcast([128, KD, 2]), op=ALU.mult),
                     reads=["gs", "ng"], writes=["gs"])


def phase_norm(g, src, dst, dst_dt, gs, sh, tag, blocks=BLOCKS, extra=None, dst_off=0, extra_alloc=None):
    k, nc = g.k, g.nc
    with ExitStack() as st:
        xin = [k.sb(st, "nx%d" % i, [128, KD, 512], F32) for i in range(2)]
        sq = k.sb(st, "nsq", [128, KD, 512], BF16)
        nhb = 1 if dst_dt == F32 else 2
        hout = [k.sb(st, "nh%d" % i, [128, KD, 512], dst_dt) for i in range(nhb)]
        rstd = k.sb(st, "nrstd", [128, 512], F32)
        ps = k.ps(st, "nps", [128, 512])
        if extra_alloc is not None:
            extra_alloc(st)
        for bi, (c0, w) in enumerate(blocks):
            s = 1 if c0 < TC else 0
            xi = xin[bi % 2]
            ho = hout[bi % nhb]
            kx = (tag + "x", bi % 2)
            kh = (tag + "h", bi % nhb)
            k.dma(None, xi[:, :, :w], fm(src, c0, w), reads=[("dram", src.tensor.name)], writes=[kx])
            k.op("act", lambda: nc.scalar.activation(out=sq[:, :, :w], in_=xi[:, :, :w], func=AF.Square), reads=[kx], writes=["nsq"])
            for kk in range(KD):
                k.op("pe", lambda: nc.tensor.matmul(ps[:, :w], lhsT=g.ones_bf[:], rhs=sq[:, kk, :w], start=(kk == 0), stop=(kk == KD - 1)),
                     reads=["nsq"], writes=["nps"])
            k.op("dve", lambda: nc.vector.tensor_scalar(out=rstd[:, :w], in0=ps[:, :w], scalar1=1.0 / D, scalar2=EPS, op0=ALU.mult, op1=ALU.add),
                 reads=["nps"], writes=["nrstd"])
            k.op("dve", lambda: nc.vector.reciprocal(out=rstd[:, :w], in_=rstd[:, :w]), reads=["nrstd"], writes=["nrstd"])
            k.op("act", lambda: nc.scalar.activation(out=rstd[:, :w], in_=rstd[:, :w], func=AF.Sqrt), reads=["nrstd"], writes=["nrstd"])
            k.op("dve", lambda: nc.vector.tensor_tensor(out=xi[:, :, :w], in0=xi[:, :, :w],
                                                        in1=rstd[:, :w].unsqueeze(1).to_broadcast([128, KD, w]), op=ALU.mult),
                 reads=[kx, "nrstd"], writes=[kx])
            for kk in range(KD):
                if sh is not None:
                    k.op("act", lambda: nc.scalar.activation(out=ho[:, kk, :w], in_=xi[:, kk, :w], func=AF.Identity,
                                                             scale=gs(kk, s), bias=sh(kk, s)),
                         reads=[kx, "mods", "gs"], writes=[kh])
                else:
                    k.op("act", lambda: nc.scalar.activation(out=ho[:, kk, :w], in_=xi[:, kk, :w], func=AF.Identity,
                                                             scale=gs(kk, s)),
                         reads=[kx, "mods", "gs"], writes=[kh])
            if dst is not None:
                k.dma(None, fm(dst, c0 - dst_off, w), ho[:, :, :w], reads=[kh], writes=[("dram", dst.tensor.name)])
            if extra is not None:
                extra(bi, c0, w, ho, kh)


def evac_kind(mi):
    if 16 <= mi < 32:
        return "sigf"
    if 32 <= mi < 40:
        return "silu"
    if 48 <= mi < 56 or mi >= 68:
        return "sig"
    return "copy"


def phase_inproj(g, l, mchunks=None):
    k, nc = g.k, g.nc
    if mchunks is None:
        mchunks = list(range(DIN // 128))
    G = 29
    groups = [mchunks[i:i + G] for i in range(0, len(mchunks), G)]
    wsrc = g.w_in[l].rearrange("(k p) c -> p k c", p=128)
    with ExitStack() as st:
        wg = k.sb(st, "ipw", [128, G, KD, 128], BF16)
        stg = [k.sb(st, "ipstg%d" % i, [128, KD, 128], F32) for i in range(2)]
        hb = [k.sb(st, "iph%d" % i, [128, KD, 512], BF16) for i in range(2)]
        ob = [k.sb(st, "ipo%d" % i, [128, 512], BF16) for i in range(4)]
        of = [k.sb(st, "ipf%d" % i, [128, 512], F32) for i in range(2)]
        ps = [k.ps(st, "ipps%d" % i, [128, 512]) for i in range(4)]
        ci = 0
        pi = 0
        oi = 0
        fi = 0
        hi = 0
        for grp in groups:
            for gi, mi in enumerate(grp):
                sg = stg[ci % 2]
                k.dma(None, sg[:], wsrc[:, :, mi * 128:(mi + 1) * 128], writes=[("ipstg", ci % 2)])
                k.op("pool", lambda: nc.gpsimd.tensor_copy(out=wg[:, gi, :, :], in_=sg[:]), reads=[("ipstg", ci % 2)], writes=[("ipw", gi)])
                ci += 1
            for (c0, w) in BLOCKS:
                h = hb[hi % 2]
                kh = ("iph", hi % 2)
                hi += 1
                k.dma(None, h[:, :, :w], fm(g.hT, c0, w), reads=[("dram", "hT")], writes=[kh])
                for gi, mi in enumerate(grp):
                    p = ps[pi % 4]
                    kp = ("ipps", pi % 4)
                    pi += 1
                    for kk in range(KD):
                        k.op("pe", lambda: nc.tensor.matmul(p[:, :w], lhsT=wg[:, gi, kk, :], rhs=h[:, kk, :w], start=(kk == 0), stop=(kk == KD - 1)),
                             reads=[kh, ("ipw", gi)], writes=[kp])
                    kind = evac_kind(mi)
                    if kind == "sigf":
                        o = of[fi % 2]
                        ko = ("ipf", fi % 2)
                        fi += 1
                        k.op("act", lambda: nc.scalar.activation(out=o[:, :w], in_=p[:, :w], func=AF.Sigmoid), reads=[kp], writes=[ko])
                        zr = (mi - 16) * 128
                        k.dma(None, g.zT[zr:zr + 128, c0:c0 + w], o[:, :w], reads=[ko], writes=[("dram", "zT")])
                    else:
                        o = ob[oi % 4]
                        ko = ("ipo", oi % 4)
                        oi += 1
                        if kind == "copy":
                            k.op("dve", lambda: nc.vector.tensor_copy(out=o[:, :w], in_=p[:, :w]), reads=[kp], writes=[ko])
                        else:
                            fn = AF.Silu if kind == "silu" else AF.Sigmoid
                            k.op("act", lambda: nc.scalar.activation(out=o[:, :w], in_=p[:, :w], func=fn), reads=[kp], writes=[ko])
                        k.dma(None, g.pT[mi * 128:(mi + 1) * 128, c0:c0 + w], o[:, :w], reads=[ko], writes=[("dram", "pT")])


def drain(k):
    for q in ("sp", "act", "pool", "pe", "dve"):
        for i, s in enumerate(k.dma_sems):
            if k.dma_val[i] > 0:
                k._wait(q, (s, k.dma_val[i]))
        for e2 in ("pe", "dve", "act", "pool"):
            if e2 != q and k.cnt[e2] > 0:
                k._wait(q, (k.sem[e2], k.cnt[e2]))


def make_program(layers=(0, 1, 2, 3), stages=None, feed=(), dump=()):
    nc = bass.Bass("TRN2", target_bir_lowering=False)
    k = K(nc)
    g = Ctx()
    g.k, g.nc = k, nc
    allst = stages is None
    g.dbg = getattr(make_program, 'dbg', False)

    def on(s):
        return allst or s in stages

    def ext_in(name, shape, dt):
        return nc.dram_tensor(name, list(shape), dt, kind="ExternalInput").ap()

    def scratch(name, shape, dt):
        if name in feed:
            return nc.dram_tensor(name, list(shape), dt, kind="ExternalInput").ap()
        if name in dump:
            return nc.dram_tensor(name, list(shape), dt, kind="ExternalOutput").ap()
        return nc.dram_tensor(name, list(shape), dt).ap()

    g.xT0 = ext_in("xT0", [D, T], F32)
    g.cvec = ext_in("cvec", [128, KD, 2], F32)
    g.b_ada = ext_in("b_ada", [128, DEPTH, 6, KD], F32)
    if on("mods"):
        g.w_ada = ext_in("w_ada", [DEPTH, D, 6 * D], F32)
    g.n1g_d = ext_in("n1g", [128, DEPTH, KD], F32)
    g.n2g_d = ext_in("n2g", [128, DEPTH, KD], F32)
    if on("inproj"):
        g.w_in = ext_in("w_in", [DEPTH, D, DIN], F32)
    g.cst_d = ext_in("cst", [128, 128 * 4], F32)
    g.lbl_d = ext_in("lbl", [128, DEPTH, 16], F32)
    g.ong_d = ext_in("ong", [128, DEPTH, 8], F32)
    g.cw_d = ext_in("cw", [128, DEPTH, 8, 31], F32)
    g.cb_d = ext_in("cb", [128, DEPTH, 8], F32)
    g.sink_d = ext_in("sink", [128, DEPTH, 8], F32)
    g.rope_d = ext_in("rope", [2, 128, TL], F32)
    g.lng_d = ext_in("lng", [128, DEPTH, 8], F32)
    g.lnb_d = ext_in("lnb", [128, DEPTH, 8], F32)
    if on("merge"):
        g.w_a = ext_in("w_a", [DEPTH, 1024, D], F32)
        g.w_b = ext_in("w_b", [DEPTH, 1024, D], F32)
        g.w_c = ext_in("w_c", [DEPTH, 1024, D], F32)
    if on("wout"):
        g.w_out = ext_in("w_out", [DEPTH, D, D], F32)
    g.w_router = ext_in("w_router", [DEPTH, D, NE], F32)
    g.ebase_d = ext_in("ebase", [128, NE], F32)
    g.fng_d = ext_in("fng", [128, KD], F32)
    if on("experts"):
        g.w_eg = ext_in("w_eg", [DEPTH, NE, D, DFF], F32)
        g.w_eu = ext_in("w_eu", [DEPTH, NE, D, DFF], F32)
        g.w_ed = ext_in("w_ed", [DEPTH, NE, DFF, D], F32)
    g.outT = nc.dram_tensor("outT", [D, TL], F32, kind="ExternalOutput").ap()

    g.hT = scratch("hT", [D, T], BF16)
    g.pT = scratch("pT", [DIN, T], BF16)
    g.zT = scratch("zT", [2048, T], F32)
    g.affd = scratch("affd", [NE, T], F32)
    g.gix = scratch("gix", [NE, T], I32)
    g.h2tok = scratch("h2tok", [T, D], BF16)
    g.xsd = scratch("xsd", [NE * SROWS, D], BF16)
    g.ysd = scratch("ysd", [NE * SROWS, D], BF16)
    g.mT = scratch("mT", [D, T], BF16)
    g.xT = scratch("xT", [D, T], F32)
    g.oT = scratch("oT", [1024, T], F32)
    g.yT = scratch("yT", [3072, T], BF16)
    g.ybT = scratch("ybT", [1024, T], F32)
    g.modsd = scratch("modsd", [128, DEPTH * 6 * KD * 2], F32)
    outs = []

    g.gsd = scratch("gsd", [2, 128, DEPTH * KD * 2], F32)
    with ExitStack() as st:
        g.pad0 = k.sb(st, "pad0_s", [128, 2048], F32)
        g.mods = k.sb(st, "mods_s", [128, DEPTH, 6, KD, 2], F32)
        g.gs1 = k.sb(st, "gs1", [128, DEPTH, KD, 2], F32)
        g.gs2 = k.sb(st, "gs2", [128, DEPTH, KD, 2], F32)
        g.n1g = k.sb(st, "n1gs", [128, DEPTH, KD], F32)
        g.n2g = k.sb(st, "n2gs", [128, DEPTH, KD], F32)
        cst = k.sb(st, "cst_s", [128, 128 * 4], F32)
        g.ones_bf = k.sb(st, "ones_bf", [128, 128], BF16)
        g.ident_bf = k.sb(st, "ident_bf", [128, 128], BF16)
        g.lb = k.sb(st, "lb_s", [128, DEPTH, 16], F32)
        g.oml = k.sb(st, "oml_s", [128, DEPTH, 16], F32)
        g.ong = k.sb(st, "ong_s", [128, DEPTH, 8], F32)
        g.cw = k.sb(st, "cw_s", [128, DEPTH, 8, 31], F32)
        g.cb = k.sb(st, "cb_s", [128, DEPTH, 8], F32)
        g.sink = k.sb(st, "sink_s", [128, DEPTH, 8], F32)
        k.dma("sp", g.ong[:], g.ong_d, writes=["ong"])
        k.dma("act", g.cw[:], g.cw_d, writes=["cw"])
        k.dma("sp", g.cb[:], g.cb_d, writes=["cw"])
        k.dma("act", g.sink[:], g.sink_d, writes=["sink"])
        g.ebase = k.sb(st, "ebase_s", [128, NE], F32)
        g.fng = k.sb(st, "fng_s", [128, KD], F32)
        zrow = k.sb(st, "zrow_s", [NE, D], BF16)
        k.dma("sp", g.ebase[:], g.ebase_d, writes=["ebase"])
        k.dma("act", g.fng[:], g.fng_d, writes=["fng"])
        k.op("dve", lambda: nc.vector.memset(zrow[:], 0.0), writes=["zrow"])
        k.dma("sp", g.ysd.rearrange("(e r) d -> e r d", r=SROWS)[:, SLOTS, :], zrow[:], reads=["zrow"], writes=[("dram", "ysd")])
        g.lng = k.sb(st, "lng_s", [128, DEPTH, 8], F32)
        g.lnb = k.sb(st, "lnb_s", [128, DEPTH, 8], F32)
        k.dma("sp", g.lng[:], g.lng_d, writes=["cw"])
        k.dma("act", g.lnb[:], g.lnb_d, writes=["cw"])
        k.dma("sp", g.n1g[:], g.n1g_d, writes=["ng"])
        k.dma("act", g.n2g[:], g.n2g_d, writes=["ng"])
        k.dma("sp", cst[:], g.cst_d, writes=["cst"])
        k.op("dve", lambda: nc.vector.memset(g.ones_bf[:], 1.0), writes=["ones"])
        k.op("dve", lambda: nc.vector.tensor_copy(out=g.ident_bf[:], in_=cst[:, 0:128]), reads=["cst"], writes=["ident"])
        g.cst = cst

        def reload():
            if not on("mods"):
                return
            k.dma("sp", g.mods[:].rearrange("p l j k s -> p (l j k s)"), g.modsd, reads=[("dram", "modsd")], writes=["mods"])
            k.dma("act", g.gs1[:].rearrange("p l k s -> p (l k s)"), g.gsd[0], reads=[("dram", "gsd")], writes=["gs"])
            k.dma("sp", g.gs2[:].rearrange("p l k s -> p (l k s)"), g.gsd[1], reads=[("dram", "gsd")], writes=["gs"])

        if on("mods"):
            phase_mods(g, layers)
            if "modsd" not in dump:
                k.dma("sp", g.modsd, g.mods[:].rearrange("p l j k s -> p (l j k s)"), reads=["mods"], writes=[("dram", "modsd")])
            k.dma("act", g.gsd[0], g.gs1[:].rearrange("p l k s -> p (l k s)"), reads=["gs"], writes=[("dram", "gsd")])
            k.dma("sp", g.gsd[1], g.gs2[:].rearrange("p l k s -> p (l k s)"), reads=["gs"], writes=[("dram", "gsd")])
            if "modsd" in dump:
                k.dma("sp", g.modsd, g.mods[:].rearrange("p l j k s -> p (l j k s)"), reads=["mods"], writes=[("dram", "modsd")])
                outs.append(("dram", "modsd"))
        if on("lb"):
            phase_lb(g)
        drain(k)
        for li, l in enumerate(layers):
            xsrc = g.xT0 if (li == 0 and "xT" not in feed) else g.xT
            if on("norm1"):
                reload()
                src = xsrc
                phase_norm(g, src, g.hT, BF16, lambda kk, s: g.gs1[:, l, kk, s:s + 1], lambda kk, s: g.mods[:, l, 0, kk, s:s + 1], "n1")
                drain(k)
            if on("inproj"):
                phase_inproj(g, l, mchunks=getattr(make_program, "mchunks", None))
                drain(k)
            if on("hgrn"):
                phase_hgrn(g, l, heads=getattr(make_program, "heads", range(8)))
                drain(k)
            if on("conv"):
                phase_conv(g, l, chunks=getattr(make_program, "cchunks", range(8)))
                drain(k)
            if on("attn"):
                phase_attn(g, l, kvheads=getattr(make_program, "kvheads", range(2)), qsub=getattr(make_program, "qsub", range(4)))
                drain(k)
            if on("merge"):
                phase_merge(g, l)
                drain(k)
            if on("wout"):
                reload()
                phase_wout(g, l, xsrc, g.xT)
                drain(k)
            if getattr(make_program, "dumpx", False):
                xm = nc.dram_tensor("xm%d" % l, [D, T], F32, kind="ExternalOutput").ap()
                for q4 in range(4):
                    k.dma(None, xm[q4 * 512:(q4 + 1) * 512, :], g.xT[q4 * 512:(q4 + 1) * 512, :], reads=[("dram", "xT")], writes=[("dram", "xm")])
                drain(k)
            if on("norm2"):
                reload()
                phase_norm2_router(g, l)
                drain(k)
            if on("route"):
                phase_route(g, l)
                drain(k)
            if on("experts"):
                phase_experts(g, l, experts=getattr(make_program, "experts", range(NE)))
                drain(k)
            if on("combine"):
                reload()
                phase_combine(g, l)
                drain(k)
            if getattr(make_program, "dumpx", False):
                xd = nc.dram_tensor("xd%d" % l, [D, T], F32, kind="ExternalOutput").ap()
                for q4 in range(4):
                    k.dma(None, xd[q4 * 512:(q4 + 1) * 512, :], g.xT[q4 * 512:(q4 + 1) * 512, :], reads=[("dram", "xT")], writes=[("dram", "xd")])
                drain(k)
        if on("final"):
            phase_norm(g, g.xT, g.outT, F32, lambda kk, s: g.fng[:, kk:kk + 1], None, "nf", blocks=BLOCKS[1:], dst_off=TC)
        drain(k)
        for e in ("pe", "dve", "act", "pool"):
            k._wait("sp", (k.sem[e], k.cnt[e])) if k.cnt[e] > 0 else None
    k.es.close()
    return nc, k


def phase_lb(g):
    k, nc = g.k, g.nc
    with ExitStack() as st:
        e = k.sb(st, "lbe", [128, DEPTH, 16], F32)
        ssum = k.sb(st, "lbs", [128, 16], F32)
        k.dma("sp", e[:], g.lbl_d, writes=["lbe"])
        k.op("act", lambda: nc.scalar.activation(out=e[:], in_=e[:], func=AF.Exp), reads=["lbe"], writes=["lbe"])
        k.op("dve", lambda: nc.vector.tensor_tensor(out=ssum[:], in0=e[:, 0, :], in1=e[:, 1, :], op=ALU.add), reads=["lbe"], writes=["lbs"])
        for l in (2, 3):
            k.op("dve", lambda: nc.vector.tensor_tensor(out=ssum[:], in0=ssum[:], in1=e[:, l, :], op=ALU.add), reads=["lbe", "lbs"], writes=["lbs"])
        k.op("dve", lambda: nc.vector.reciprocal(out=ssum[:], in_=ssum[:]), reads=["lbs"], writes=["lbs"])
        lb = g.lb
        k.op("dve", lambda: nc.vector.memset(lb[:, 0, :], 0.0), writes=["lb"])
        for l in (1, 2, 3):
            k.op("dve", lambda: nc.vector.tensor_tensor(out=e[:, l, :], in0=e[:, l, :], in1=ssum[:], op=ALU.mult), reads=["lbe", "lbs"], writes=["lbe"])
            k.op("dve", lambda: nc.vector.tensor_tensor(out=lb[:, l, :], in0=lb[:, l - 1, :], in1=e[:, l, :], op=ALU.add), reads=["lbe", "lb"], writes=["lb"])
        k.op("dve", lambda: nc.vector.tensor_scalar(out=g.oml[:], in0=lb[:], scalar1=-1.0, scalar2=1.0, op0=ALU.mult, op1=ALU.add),
             reads=["lb"], writes=["oml"])


HSEGS = [(0, TC)] + [(TC + 2048 * i, 2048) for i in range(TL // 2048)]


def phase_hgrn(g, l, heads=range(8)):
    k, nc = g.k, g.nc
    CH = 64
    with ExitStack() as st:
        WM = 2048
        q = k.sb(st, "hq", [128, WM], BF16)
        v = k.sb(st, "hv", [128, WM], BF16)
        gsl = k.sb(st, "hg", [128, WM], BF16)
        f = k.sb(st, "hf", [128, WM], F32)
        kk = k.sb(st, "hkk", [128, WM], F32)
        lf = k.sb(st, "hlf", [128, WM], F32)
        b = k.sb(st, "hb", [128, WM], F32)
        bx = k.sb(st, "hbx", [128, WM], F32)
        dd = k.sb(st, "hd", [128, WM], F32)
        E = k.sb(st, "hE", [128, WM], F32)
        qd = k.sb(st, "hqd", [128, WM], BF16)
        kd = k.sb(st, "hkd", [128, WM], BF16)
        qin = k.sb(st, "hqin", [128, WM], BF16)
        kend = k.sb(st, "hkend", [128, WM], BF16)
        dec = k.sb(st, "hdec", [128, WM // CH], F32)
        rmask = k.sb(st, "hrm", [128, WM], F32)
        qd2 = k.sb(st, "hqd2", [128, WM], BF16)
        kd2 = k.sb(st, "hkd2", [128, WM], BF16)
        vtok = k.sb(st, "hvtok", [32, WM // CH, 2, 128], BF16)
        ktok = k.sb(st, "hktok", [32, WM // CH, 2, 128], BF16)
        am = k.sb(st, "ham", [32, WM // CH, 2, CH], BF16)
        osb = k.sb(st, "hosb", [128, WM], F32)
        ofw = k.sb(st, "hofw", [128, WM], F32)
        sq = k.sb(st, "hsq", [128, WM], BF16)
        rs = k.sb(st, "hrs", [128, 512], F32)
        yout = k.sb(st, "hy", [128, WM], BF16)
        S32 = k.sb(st, "hS32", [128, 128], F32)
        Sbf = [k.sb(st, "hSbf%d" % i, [128, 128], BF16) for i in range(2)]
        mAf = k.sb(st, "hmAf", [32, 64], BF16)
        mBb = k.sb(st, "hmBb", [32, 64], BF16)
        ptv = k.ps(st, "hptv", [32, 4, 2, 128], BF16)
        ptk = k.ps(st, "hptk", [32, 4, 2, 128], BF16)
        pat = k.ps(st, "hpat", [32, 4, 2, CH])
        pkv = [k.ps(st, "hpkv%d" % i, [128, 4, 128]) for i in range(2)]
        po = [k.ps(st, "hpo%d" % i, [128, 512]) for i in range(2)]
        k.op("dve", lambda: nc.vector.memset(rmask[:], 1.0), writes=["hrm"])
        k.op("dve", lambda: nc.vector.memset(rmask[:].rearrange("p (c j) -> p c j", j=CH)[:, :, 0:1], 0.0), writes=["hrm"])
        k.op("dve", lambda: nc.vector.memset(mAf[:], 1.0), writes=["hmask"])
        k.op("dve", lambda: nc.vector.memset(mBb[:], 1.0), writes=["hmask"])
        k.op("dve", lambda: nc.vector.tensor_copy(out=mAf[:, 0:32], in_=g.cst[0:32, 128:160]), reads=["cst"], writes=["hmask"])
        k.op("dve", lambda: nc.vector.tensor_copy(out=mBb[:, 32:64], in_=g.cst[0:32, 256:288]), reads=["cst"], writes=["hmask"])
        sbi = 0
        heads = list(heads)
        for h in [heads[0]] + heads:
            for di in (0, 1):
                segs = HSEGS if di == 0 else [HSEGS[0]] + HSEGS[:0:-1]
                zrow = (di * 8 + h) * 128
                lbi = di * 8 + h
                k.op("dve", lambda: nc.vector.memset(S32[:], 0.0), writes=["hS32"])
                k.op("pool", lambda: nc.gpsimd.memset(am[:], 0.0), writes=[("ham", i) for i in range(WM // CH // 4)])
                k.op("dve", lambda: nc.vector.memset(Sbf[sbi % 2][:], 0.0), writes=[("hSbf", sbi % 2)])
                for (c0, W) in segs:
                    NCH = W // CH
                    c3 = lambda t: t[:, :W].rearrange("p (c j) -> p c j", j=CH)
                    k.dma(None, q[:, :W], g.pT[h * 128:(h + 1) * 128, c0:c0 + W], reads=[("dram", "pT")], writes=["hq"])
                    k.dma(None, v[:, :W], g.pT[(8 + h) * 128:(9 + h) * 128, c0:c0 + W], reads=[("dram", "pT")], writes=["hv"])
                    k.dma(None, f[:, :W], g.zT[zrow:zrow + 128, c0:c0 + W], reads=[("dram", "zT")], writes=["hf"])
                    if di == 1:
                        k.dma(None, gsl[:, :W], g.pT[(32 + h) * 128:(33 + h) * 128, c0:c0 + W], reads=[("dram", "pT")], writes=["hg"])
                        k.dma(None, ofw[:, :W], g.oT[h * 128:(h + 1) * 128, c0:c0 + W], reads=[("dram", "oT")], writes=["hofw"])
                    k.op("dve", lambda: nc.vector.tensor_scalar(out=f[:, :W], in0=f[:, :W], scalar1=g.oml[:, l, lbi:lbi + 1], scalar2=g.lb[:, l, lbi:lbi + 1],
                                                                op0=ALU.mult, op1=ALU.add), reads=["hf", "lb", "oml"], writes=["hf"])
                    k.op("dve", lambda: nc.vector.tensor_scalar(out=kk[:, :W], in0=f[:, :W], scalar1=-1.0, scalar2=1.0, op0=ALU.mult, op1=ALU.add),
                         reads=["hf"], writes=["hkk"])
                    k.op("act", lambda: nc.scalar.activation(out=lf[:, :W], in_=f[:, :W], func=AF.Ln), reads=["hf"], writes=["hlf"])
                    k.op("dve", lambda: nc.vector.tensor_tensor_scan(out=b[:, :W], data0=rmask[:, :W], data1=lf[:, :W], initial=0.0, op0=ALU.mult, op1=ALU.add),
                         reads=["hlf", "hrm"], writes=["hb"])
                    if di == 0:
                        bxx = b
                        kbx = "hb"
                        mid, end = 31, 63
                    else:
                        k.op("dve", lambda: nc.vector.tensor_tensor(out=c3(dd), in0=c3(b), in1=c3(b)[:, :, 63:64].to_broadcast([128, NCH, CH]), op=ALU.subtract),
                             reads=["hb"], writes=["hd"])
                        k.op("dve", lambda: nc.vector.tensor_tensor(out=bx[:, :W], in0=lf[:, :W], in1=dd[:, :W], op=ALU.subtract),
                             reads=["hlf", "hd"], writes=["hbx"])
                        bxx = bx
                        kbx = "hbx"
                        mid, end = 32, 0
                    c4v = lambda t: t[:, :W].rearrange("p (c s j) -> p c s j", s=2, j=32)
                    k.op("dve", lambda: nc.vector.tensor_tensor(out=c4v(dd), in0=c4v(bxx), in1=c4v(bxx)[:, :, :, 15:16].to_broadcast([128, NCH, 2, 32]), op=ALU.subtract),
                         reads=[kbx], writes=["hd"])
                    k.op("act", lambda: nc.scalar.activation(out=E[:, :W], in_=dd[:, :W], func=AF.Exp), reads=["hd"], writes=["hE"])
                    k.op("dve", lambda: nc.vector.tensor_tensor(out=qd[:, :W], in0=q[:, :W], in1=E[:, :W], op=ALU.mult), reads=["hq", "hE"], writes=["hqd"])
                    k.op("act", lambda: nc.scalar.activation(out=E[:, :W], in_=dd[:, :W], func=AF.Exp, scale=-1.0), reads=["hd"], writes=["hE"])
                    k.op("dve", lambda: nc.vector.tensor_tensor(out=kd[:, :W], in0=kk[:, :W], in1=E[:, :W], op=ALU.mult), reads=["hkk", "hE"], writes=["hkd"])
                    k.op("dve", lambda: nc.vector.tensor_tensor(out=c3(dd), in0=c3(bxx), in1=c3(bxx)[:, :, mid:mid + 1].to_broadcast([128, NCH, CH]), op=ALU.subtract),
                         reads=[kbx], writes=["hd"])
                    k.op("act", lambda: nc.scalar.activation(out=E[:, :W], in_=dd[:, :W], func=AF.Exp), reads=["hd"], writes=["hE"])
                    k.op("dve", lambda: nc.vector.tensor_tensor(out=qd2[:, :W], in0=q[:, :W], in1=E[:, :W], op=ALU.mult), reads=["hq", "hE"], writes=["hqd2"])
                    k.op("act", lambda: nc.scalar.activation(out=E[:, :W], in_=dd[:, :W], func=AF.Exp, scale=-1.0), reads=["hd"], writes=["hE"])
                    k.op("dve", lambda: nc.vector.tensor_tensor(out=kd2[:, :W], in0=kk[:, :W], in1=E[:, :W], op=ALU.mult), reads=["hkk", "hE"], writes=["hkd2"])
                    k.op("act", lambda: nc.scalar.activation(out=E[:, :W], in_=bxx[:, :W], func=AF.Exp), reads=[kbx], writes=["hE"])
                    k.op("dve", lambda: nc.vector.tensor_tensor(out=qin[:, :W], in0=q[:, :W], in1=E[:, :W], op=ALU.mult), reads=["hq", "hE"], writes=["hqin"])
                    k.op("dve", lambda: nc.vector.tensor_tensor(out=c3(dd), in0=c3(bxx), in1=c3(bxx)[:, :, end:end + 1].to_broadcast([128, NCH, CH]), op=ALU.subtract),
                         reads=[kbx], writes=["hd"])
                    k.op("act", lambda: nc.scalar.activation(out=E[:, :W], in_=dd[:, :W], func=AF.Exp, scale=-1.0), reads=["hd"], writes=["hE"])
                    k.op("dve", lambda: nc.vector.tensor_tensor(out=kend[:, :W], in0=kk[:, :W], in1=E[:, :W], op=ALU.mult), reads=["hkk", "hE"], writes=["hkend"])
                    k.op("act", lambda: nc.scalar.activation(out=dec[:, :NCH], in_=c3(bxx)[:, :, end], func=AF.Exp), reads=[kbx], writes=["hdec"])
                    for c4 in range(0, NCH, 4):
                        for j in range(4):
                            for sbk in range(2):
                                cs = (c4 + j) * CH + sbk * 32
                                k.op("pe", lambda: nc.tensor.transpose(out=ptv[:, j, sbk, :], in_=v[:, cs:cs + 32], identity=g.ident_bf[:]), reads=["hv", "ident"], writes=["hptv"])
                                k.op("pe", lambda: nc.tensor.transpose(out=ptk[:, j, sbk, :], in_=kend[:, cs:cs + 32], identity=g.ident_bf[:]), reads=["hkend", "ident"], writes=["hptk"])
                        k.op("dve", lambda: nc.vector.tensor_copy(out=vtok[:, c4:c4 + 4, :, :], in_=ptv[:]), reads=["hptv"], writes=[("hvtok", c4 // 4)])
                        k.op("act", lambda: nc.scalar.copy(out=ktok[:, c4:c4 + 4, :, :], in_=ptk[:]), reads=["hptk"], writes=[("hktok", c4 // 4)])
                        for j in range(4):
                            ca = (c4 + j) * CH
                            cb_ = ca + 32
                            mm = lambda o, lh, rh: k.op("pe", lambda: nc.tensor.matmul(o, lhsT=lh, rhs=rh, start=True, stop=True),
                                                         reads=["hkd", "hqd", "hkd2", "hqd2"], writes=["hpat"])
                            mm(pat[:, j, 0, 0:32], kd[:, ca:ca + 32], qd[:, ca:ca + 32])
                            mm(pat[:, j, 1, 32:64], kd[:, cb_:cb_ + 32], qd[:, cb_:cb_ + 32])
                            if di == 0:
                                mm(pat[:, j, 0, 32:64], kd2[:, ca:ca + 32], qd2[:, cb_:cb_ + 32])
                            else:
                                mm(pat[:, j, 1, 0:32], kd2[:, cb_:cb_ + 32], qd2[:, ca:ca + 32])
                        if di == 0:
                            k.op("dve", lambda: nc.vector.tensor_tensor(out=am[:, c4:c4 + 4, 0, :], in0=pat[:, :, 0, :], in1=mAf[:].unsqueeze(1).to_broadcast([32, 4, 64]), op=ALU.mult),
                                 reads=["hpat", "hmask"], writes=[("ham", c4 // 4)])
                            k.op("dve", lambda: nc.vector.tensor_tensor(out=am[:, c4:c4 + 4, 1, 32:64], in0=pat[:, :, 1, 32:64], in1=mAf[:, 0:32].unsqueeze(1).to_broadcast([32, 4, 32]), op=ALU.mult),
                                 reads=["hpat", "hmask"], writes=[("ham", c4 // 4)])
                        else:
                            k.op("dve", lambda: nc.vector.tensor_tensor(out=am[:, c4:c4 + 4, 1, :], in0=pat[:, :, 1, :], in1=mBb[:].unsqueeze(1).to_broadcast([32, 4, 64]), op=ALU.mult),
                                 reads=["hpat", "hmask"], writes=[("ham", c4 // 4)])
                            k.op("dve", lambda: nc.vector.tensor_tensor(out=am[:, c4:c4 + 4, 0, 0:32], in0=pat[:, :, 0, 0:32], in1=mBb[:, 32:64].unsqueeze(1).to_broadcast([32, 4, 32]), op=ALU.mult),
                                 reads=["hpat", "hmask"], writes=[("ham", c4 // 4)])
                    order = list(range(NCH)) if di == 0 else list(range(NCH - 1, -1, -1))
                    for oi, c in enumerate(order):
                        cs = c * CH
                        pk = pkv[(oi // 4) % 2]
                        kpk = ("hpkv", (oi // 4) % 2)
                        for sbk in range(2):
                            k.op("pe", lambda: nc.tensor.matmul(pk[:, oi % 4, :], lhsT=ktok[:, c, sbk, :], rhs=vtok[:, c, sbk, :], start=(sbk == 0), stop=(sbk == 1)),
                                 reads=[("hktok", c // 4), ("hvtok", c // 4)], writes=[kpk])
                        pb = po[(c // 8) % 2]
                        kpo = ("hpo", (c // 8) % 2)
                        oc = (c % 8) * CH
                        for sbk in range(2):
                            k.op("pe", lambda: nc.tensor.matmul(pb[:, oc:oc + CH], lhsT=vtok[:, c, sbk, :], rhs=am[:, c, sbk, :], start=(sbk == 0), stop=False),
                                 reads=[("hvtok", c // 4), ("ham", c // 4)], writes=[kpo])
                        k.op("pe", lambda: nc.tensor.matmul(pb[:, oc:oc + CH], lhsT=Sbf[sbi % 2][:], rhs=qin[:, cs:cs + CH], start=False, stop=True),
                             reads=[("hSbf", sbi % 2), "hqin"], writes=[kpo])
                        k.op("dve", lambda: nc.vector.scalar_tensor_tensor(out=S32[:], in0=S32[:], scalar=dec[:, c:c + 1], in1=pk[:, oi % 4, :], op0=ALU.mult, op1=ALU.add),
                             reads=["hS32", "hdec", kpk], writes=["hS32"])
                        sbi += 1
                        k.op("pool", lambda: nc.gpsimd.tensor_copy(out=Sbf[sbi % 2][:], in_=S32[:]), reads=["hS32"], writes=[("hSbf", sbi % 2)])
                        last_in_bank = (c % 8 == 7) if di == 0 else (c % 8 == 0)
                        if last_in_bank or NCH < 8 and oi == NCH - 1:
                            b0 = (c // 8) * 512
                            bw = min(512, W - b0)
                            k.op("act", lambda: nc.scalar.copy(out=osb[:, b0:b0 + bw], in_=pb[:, :bw]), reads=[kpo], writes=["hosb"])
                    if getattr(g, "dbg", False) and di == 0 and h == 0:
                        for ri, (tt, kt) in enumerate(((f, "hf"), (lf, "hlf"), (b, "hb"), (kk, "hkk"), (E, "hE"), (osb, "hosb"))):
                            k.dma("sp", g.ybT[ri * 128:(ri + 1) * 128, c0:c0 + W], tt[:, :W], reads=[kt], writes=[("dram", "ybT")])
                        k.dma("sp", g.ybT[768:896, 0:16], g.lb[:, l, :], reads=["lb"], writes=[("dram", "ybT")])
                        k.dma("sp", g.ybT[896:1024, 0:128], S32[:], reads=["hS32"], writes=[("dram", "ybT")])
                    if di == 0:
                        k.dma(None, g.oT[h * 128:(h + 1) * 128, c0:c0 + W], osb[:, :W], reads=["hosb"], writes=[("dram", "oT")])
                    else:
                        k.op("dve", lambda: nc.vector.tensor_tensor(out=osb[:, :W], in0=osb[:, :W], in1=ofw[:, :W], op=ALU.add), reads=["hosb", "hofw"], writes=["hosb"])
                        k.op("act", lambda: nc.scalar.activation(out=sq[:, :W], in_=osb[:, :W], func=AF.Square), reads=["hosb"], writes=["hsq"])
                        for b0 in range(0, W, 512):
                            bw = min(512, W - b0)
                            pb = po[(b0 // 512) % 2]
                            kpo = ("hpo", (b0 // 512) % 2)
                            k.op("pe", lambda: nc.tensor.matmul(pb[:, :bw], lhsT=g.ones_bf[:], rhs=sq[:, b0:b0 + bw], start=True, stop=True), reads=["hsq", "ones"], writes=[kpo])
                            k.op("dve", lambda: nc.vector.tensor_scalar(out=rs[:, :bw], in0=pb[:, :bw], scalar1=1.0 / 128, scalar2=EPS, op0=ALU.mult, op1=ALU.add),
                                 reads=[kpo], writes=["hrs"])
                            k.op("dve", lambda: nc.vector.reciprocal(out=rs[:, :bw], in_=rs[:, :bw]), reads=["hrs"], writes=["hrs"])
                            k.op("act", lambda: nc.scalar.activation(out=rs[:, :bw], in_=rs[:, :bw], func=AF.Sqrt), reads=["hrs"], writes=["hrs"])
                            k.op("dve", lambda: nc.vector.tensor_tensor(out=osb[:, b0:b0 + bw], in0=osb[:, b0:b0 + bw], in1=rs[:, :bw], op=ALU.mult),
                                 reads=["hosb", "hrs"], writes=["hosb"])
                        k.op("dve", lambda: nc.vector.scalar_tensor_tensor(out=yout[:, :W], in0=osb[:, :W], scalar=g.ong[:, l, h:h + 1], in1=gsl[:, :W], op0=ALU.mult, op1=ALU.mult),
                             reads=["hosb", "hg", "ong"], writes=["hy"])
                        k.dma(None, g.yT[h * 128:(h + 1) * 128, c0:c0 + W], yout[:, :W], reads=["hy"], writes=[("dram", "yT")])
                if di == 0:
                    drain(k)


def phase_conv(g, l, chunks=range(8)):
    k, nc = g.k, g.nc
    PAD = 15
    with ExitStack() as st:
        a = k.sb(st, "cva", [128, T], BF16)
        sg = k.sb(st, "cvs", [128, T], BF16)
        hc = k.sb(st, "cvhc", [128, TC + 2 * PAD], BF16)
        hl = k.sb(st, "cvhl", [128, TL + 2 * PAD], BF16)
        dg = k.sb(st, "cvdg", [128, 31, 128], BF16)
        ot = [k.sb(st, "cvo%d" % i, [128, 512], F32) for i in range(2)]
        ps = [k.ps(st, "cvps%d" % i, [128, 512]) for i in range(2)]
        k.op("pool", lambda: nc.gpsimd.memset(hc[:], 0.0), writes=["cvhc"])
        k.op("pool", lambda: nc.gpsimd.memset(hl[:], 0.0), writes=["cvhl"])
        bi = 0
        for j in chunks:
            k.dma(None, a[:], g.pT[(40 + j) * 128:(41 + j) * 128, :], reads=[("dram", "pT")], writes=["cva"])
            k.dma(None, sg[:], g.pT[(48 + j) * 128:(49 + j) * 128, :], reads=[("dram", "pT")], writes=["cvs"])
            k.op("dve", lambda: nc.vector.tensor_tensor(out=hc[:, PAD:PAD + TC], in0=a[:, :TC], in1=sg[:, :TC], op=ALU.mult), reads=["cva", "cvs"], writes=["cvhc"])
            k.op("pool", lambda: nc.gpsimd.tensor_tensor(out=hl[:, PAD:PAD + TL], in0=a[:, TC:], in1=sg[:, TC:], op=ALU.mult), reads=["cva", "cvs"], writes=["cvhl"])
            for tap in range(31):
                k.op("dve", lambda: nc.vector.tensor_scalar(out=dg[:, tap, :], in0=g.ident_bf[:], scalar1=g.cw[:, l, j, tap:tap + 1], scalar2=None, op0=ALU.mult),
                     reads=["ident", "cw"], writes=["cvdg"])
            for (c0, w) in BLOCKS:
                src, t0, ks = (hc, c0, "cvhc") if c0 < TC else (hl, c0 - TC, "cvhl")
                p = ps[bi % 2]
                o = ot[bi % 2]
                for tap in range(31):
                    k.op("pe", lambda: nc.tensor.matmul(p[:, :w], lhsT=dg[:, tap, :], rhs=src[:, t0 + tap:t0 + tap + w], start=(tap == 0), stop=(tap == 30)),
                         reads=["cvdg", ks], writes=[("cvps", bi % 2)])
                k.op("act", lambda: nc.scalar.activation(out=o[:, :w], in_=p[:, :w], func=AF.Identity, bias=g.cb[:, l, j:j + 1]),
                     reads=[("cvps", bi % 2), "cw"], writes=[("cvo", bi % 2)])
                k.dma(None, g.ybT[j * 128:(j + 1) * 128, c0:c0 + w], o[:, :w], reads=[("cvo", bi % 2)], writes=[("dram", "ybT")])
                bi += 1


def phase_attn(g, l, kvheads=range(2), qsub=range(4)):
    k, nc = g.k, g.nc
    NLT = TL // 128
    scale = 128 ** -0.5
    with ExitStack() as st:
        cosT = k.sb(st, "atcos", [128, TL], F32)
        sinT = k.sb(st, "atsin", [128, TL], F32)
        rm = k.sb(st, "atrm", [128, 128], BF16)
        mge = k.sb(st, "atmge", [128, 128], BF16)
        mle = k.sb(st, "atmle", [128, 128], BF16)
        kr = k.sb(st, "atk", [128, T], BF16)
        vT = k.sb(st, "atv", [128, T], BF16)
        qr = k.sb(st, "atq", [128, T], BF16)
        vtok = k.sb(st, "atvtok", [128, T // 128, 128], BF16)
        t1 = k.sb(st, "att1", [128, 512], F32)
        t2 = k.sb(st, "att2", [128, 512], F32)
        pts = [k.sb(st, "atp%d" % i, [128, 5, 128], BF16) for i in range(2)]
        rden = k.sb(st, "atrden", [128, 128], F32)
        yc = k.sb(st, "atyc", [128, T], BF16)
        esk = k.sb(st, "atesk", [128, 8], F32)
        prot = k.ps(st, "atprot", [128, 512])
        ptr = k.ps(st, "atptr", [128, 4, 128], BF16)
        psl = [k.ps(st, "atpsl%d" % i, [128, 3, 128]) for i in range(2)]
        psc = [k.ps(st, "atpsc%d" % i, [128, 2, 128]) for i in range(2)]
        pso = [k.ps(st, "atpso%d" % i, [128, 2, 128]) for i in range(2)]
        k.dma("sp", cosT[:], g.rope_d[0], writes=["atcos"])
        k.dma("act", sinT[:], g.rope_d[1], writes=["atsin"])
        k.op("dve", lambda: nc.vector.tensor_copy(out=rm[:], in_=g.cst[:, 384:512]), reads=["cst"], writes=["atrm"])
        k.op("dve", lambda: nc.vector.tensor_copy(out=mle[:], in_=g.cst[:, 128:256]), reads=["cst"], writes=["atm"])
        k.op("dve", lambda: nc.vector.tensor_copy(out=mge[:], in_=g.cst[:, 256:384]), reads=["cst"], writes=["atm"])
        k.op("act", lambda: nc.scalar.activation(out=esk[:], in_=g.sink[:, l, :], func=AF.Exp), reads=["sink"], writes=["atesk"])

        def rope(dst, src, ksrc, kdst):
            for b0 in range(0, TL, 512):
                xs = src[:, TC + b0:TC + b0 + 512]
                k.op("pe", lambda: nc.tensor.matmul(prot[:], lhsT=rm[:], rhs=xs, start=True, stop=True), reads=[ksrc, "atrm"], writes=["atprot"])
                k.op("dve", lambda: nc.vector.tensor_tensor(out=t1[:], in0=prot[:], in1=sinT[:, b0:b0 + 512], op=ALU.mult), reads=["atprot", "atsin"], writes=["att1"])
                k.op("pool", lambda: nc.gpsimd.tensor_tensor(out=t2[:], in0=xs, in1=cosT[:, b0:b0 + 512], op=ALU.mult), reads=[ksrc, "atcos"], writes=["att2"])
                k.op("dve", lambda: nc.vector.tensor_tensor(out=dst[:, TC + b0:TC + b0 + 512], in0=t1[:], in1=t2[:], op=ALU.add), reads=["att1", "att2"], writes=[kdst])

        bi = 0
        for kvh in kvheads:
            k.dma(None, kr[:], g.pT[(64 + kvh) * 128:(65 + kvh) * 128, :], reads=[("dram", "pT")], writes=["atk"])
            k.dma(None, vT[:], g.pT[(66 + kvh) * 128:(67 + kvh) * 128, :], reads=[("dram", "pT")], writes=["atv"])
            rope(kr, kr, "atk", "atk")
            for t4 in range(0, T // 128, 4):
                n4 = min(4, T // 128 - t4)
                for j in range(n4):
                    cs = (t4 + j) * 128
                    k.op("pe", lambda: nc.tensor.transpose(out=ptr[:, j, :], in_=vT[:, cs:cs + 128], identity=g.ident_bf[:]), reads=["atv", "ident"], writes=["atptr"])
                k.op("dve", lambda: nc.vector.tensor_copy(out=vtok[:, t4:t4 + n4, :], in_=ptr[:, :n4, :]), reads=["atptr"], writes=["atvtok"])
            for qs in qsub:
                h = kvh * 4 + qs
                k.dma(None, qr[:], g.pT[(56 + h) * 128:(57 + h) * 128, :], reads=[("dram", "pT")], writes=["atq"])
                rope(qr, qr, "atq", "atq")
                qblocks = [("lat", n) for n in range(NLT)] + [("ctx", n) for n in range(TC // 128)]
                for (kind, n) in qblocks:
                    pt = pts[bi % 2]
                    kpt = ("atp", bi % 2)
                    sl, sc, so = psl[bi % 2], psc[bi % 2], pso[bi % 2]
                    ksl, ksc, kso = ("atpsl", bi % 2), ("atpsc", bi % 2), ("atpso", bi % 2)
                    bi += 1
                    if kind == "lat":
                        qc = TC + 128 * n
                        loc = [(s, n - 1 + s) for s in range(3) if 0 <= n - 1 + s < NLT]
                    else:
                        qc = 128 * n
                        loc = []
                    qv = qr[:, qc:qc + 128]
                    for (s, tn) in loc:
                        kc = TC + 128 * tn
                        k.op("pe", lambda: nc.tensor.matmul(sl[:, s, :], lhsT=kr[:, kc:kc + 128], rhs=qv, start=True, stop=True), reads=["atk", "atq"], writes=[ksl])
                    for s in range(2):
                        k.op("pe", lambda: nc.tensor.matmul(sc[:, s, :], lhsT=kr[:, 128 * s:128 * s + 128], rhs=qv, start=True, stop=True), reads=["atk", "atq"], writes=[ksc])
                    if loc:
                        s0, s1 = loc[0][0], loc[-1][0] + 1
                        k.op("act", lambda: nc.scalar.activation(out=pt[:, s0:s1, :], in_=sl[:, s0:s1, :], func=AF.Exp, scale=scale), reads=[ksl], writes=[kpt])
                        if s0 == 0:
                            k.op("pool", lambda: nc.gpsimd.tensor_tensor(out=pt[:, 0, :], in0=pt[:, 0, :], in1=mge[:], op=ALU.mult), reads=[kpt, "atm"], writes=[kpt])
                        if s1 == 3:
                            k.op("pool", lambda: nc.gpsimd.tensor_tensor(out=pt[:, 2, :], in0=pt[:, 2, :], in1=mle[:], op=ALU.mult), reads=[kpt, "atm"], writes=[kpt])
                    k.op("act", lambda: nc.scalar.activation(out=pt[:, 3:5, :], in_=sc[:], func=AF.Exp, scale=scale), reads=[ksc], writes=[kpt])
                    tiles = [(s, (TC // 128) + tn) for (s, tn) in loc] + [(3, 0), (4, 1)]
                    for i, (s, vt) in enumerate(tiles):
                        k.op("pe", lambda: nc.tensor.matmul(so[:, 0, :], lhsT=vtok[:, vt, :], rhs=pt[:, s, :], start=(i == 0), stop=(i == len(tiles) - 1)),
                             reads=["atvtok", kpt], writes=[kso])
                    for i, (s, vt) in enumerate(tiles):
                        k.op("pe", lambda: nc.tensor.matmul(so[:, 1, :], lhsT=g.ones_bf[:], rhs=pt[:, s, :], start=(i == 0), stop=(i == len(tiles) - 1)),
                             reads=["ones", kpt], writes=[kso])
                    k.op("dve", lambda: nc.vector.tensor_scalar(out=rden[:], in0=so[:, 1, :], scalar1=esk[:, h:h + 1], scalar2=None, op0=ALU.add), reads=[kso, "atesk"], writes=["atrden"])
                    k.op("dve", lambda: nc.vector.reciprocal(out=rden[:], in_=rden[:]), reads=["atrden"], writes=["atrden"])
                    k.op("dve", lambda: nc.vector.tensor_tensor(out=yc[:, qc:qc + 128], in0=so[:, 0, :], in1=rden[:], op=ALU.mult), reads=[kso, "atrden"], writes=["atyc"])
                k.dma(None, g.yT[(16 + h) * 128:(17 + h) * 128, :], yc[:], reads=["atyc"], writes=[("dram", "yT")])


def _pk(a):
    sh = a.shape
    return np.ascontiguousarray(np.moveaxis(a.reshape(sh[:-1] + (sh[-1] // 128, 128)), -1, 0))


def const_tables():
    cst = np.zeros((128, 512), np.float32)
    p = np.arange(128)[:, None]
    j = np.arange(128)[None, :]
    cst[:, 0:128] = (p == j)
    cst[:, 128:256] = (p <= j)
    cst[:, 256:384] = (p >= j)
    rm = np.zeros((128, 128), np.float32)
    for do in range(128):
        i = do % 64
        if i < 32:
            rm[do + 32, do] = -1.0
        else:
            rm[do - 32, do] = 1.0
    cst[:, 384:512] = rm
    pos = np.arange(TL)
    rows = (pos // 64).astype(np.float32)
    cols = (pos % 64).astype(np.float32)
    inv = (10000.0 ** (-(np.arange(0, 64, 2, dtype=np.float32)) / 64.0)).astype(np.float32)
    rope = np.zeros((2, 128, TL), np.float32)
    for d in range(128):
        base = rows if d < 64 else cols
        ang = (base * inv[(d % 64) % 32]).astype(np.float32)
        rope[0, d] = np.cos(ang)
        rope[1, d] = np.sin(ang)
    return cst, rope


def host_inputs(inp):
    m = {}
    x = np.asarray(inp["x"], np.float32)[0]
    ctx = np.asarray(inp["ctx"], np.float32)[0]
    m["xT0"] = np.ascontiguousarray(np.concatenate([ctx, x], 0).T)
    cc = np.stack([np.asarray(inp["c"], np.float32)[0], np.asarray(inp["c_ctx"], np.float32)], -1)
    m["cvec"] = np.ascontiguousarray(cc.reshape(KD, 128, 2).transpose(1, 0, 2))
    m["b_ada"] = np.ascontiguousarray(np.asarray(inp["b_ada"], np.float32).reshape(DEPTH, 6, KD, 128).transpose(3, 0, 1, 2))
    m["w_ada"] = np.asarray(inp["w_ada"], np.float32)
    m["n1g"] = np.ascontiguousarray(np.asarray(inp["norm1_g"], np.float32).reshape(DEPTH, KD, 128).transpose(2, 0, 1))
    m["n2g"] = np.ascontiguousarray(np.asarray(inp["norm2_g"], np.float32).reshape(DEPTH, KD, 128).transpose(2, 0, 1))
    m["w_in"] = np.asarray(inp["w_in"], np.float32)
    cst, rope = const_tables()
    m["cst"] = cst
    m["rope"] = rope
    m["lbl"] = np.ascontiguousarray(np.asarray(inp["hgrn_lb_logits"], np.float32).reshape(DEPTH, 2, 8, 128).transpose(3, 0, 1, 2).reshape(128, DEPTH, 16))
    m["ong"] = np.ascontiguousarray(np.asarray(inp["hgrn_onorm_g"], np.float32).reshape(DEPTH, 8, 128).transpose(2, 0, 1))
    m["cw"] = np.ascontiguousarray(np.asarray(inp["conv_w"], np.float32).reshape(DEPTH, 31, 8, 128).transpose(3, 0, 2, 1))
    m["cb"] = np.ascontiguousarray(np.asarray(inp["conv_b"], np.float32).reshape(DEPTH, 8, 128).transpose(2, 0, 1))
    m["lng"] = np.ascontiguousarray(np.asarray(inp["conv_ln_g"], np.float32).reshape(DEPTH, 8, 128).transpose(2, 0, 1))
    m["lnb"] = np.ascontiguousarray(np.asarray(inp["conv_ln_b"], np.float32).reshape(DEPTH, 8, 128).transpose(2, 0, 1))
    m["w_a"] = np.asarray(inp["w_branch_a"], np.float32)
    m["w_b"] = np.asarray(inp["w_branch_b"], np.float32)
    m["w_c"] = np.asarray(inp["w_branch_c"], np.float32)
    m["w_out"] = np.asarray(inp["w_out"], np.float32)
    m["w_router"] = np.asarray(inp["w_router"], np.float32)
    m["ebase"] = np.ascontiguousarray(np.broadcast_to((np.arange(NE, dtype=np.float32) * SROWS + SLOTS)[None], (128, NE)))
    m["fng"] = np.ascontiguousarray(np.asarray(inp["final_norm_g"], np.float32).reshape(KD, 128).T)
    m["w_eg"] = np.asarray(inp["w_exp_gate"], np.float32)
    m["w_eu"] = np.asarray(inp["w_exp_up"], np.float32)
    m["w_ed"] = np.asarray(inp["w_exp_down"], np.float32)
    m["sink"] = np.ascontiguousarray(np.broadcast_to(np.asarray(inp["attn_sink"], np.float32)[None], (128, DEPTH, 8)))
    return m


def phase_merge(g, l):
    k, nc = g.k, g.nc
    with ExitStack() as st:
        wb = [k.sb(st, "mgw%d" % i, [128, 8, D], BF16) for i in range(3)]
        stg = [k.sb(st, "mgstg%d" % i, [128, D], F32) for i in range(2)]
        ya = [k.sb(st, "mgya%d" % i, [128, 8, 512], BF16) for i in range(1)]
        yc = [k.sb(st, "mgyc%d" % i, [128, 8, 512], BF16) for i in range(1)]
        yb32 = k.sb(st, "mgyb32", [128, 8, 512], F32)
        ybs = k.sb(st, "mgybs", [128, 8, 512], BF16)
        yb = k.sb(st, "mgyb", [128, 8, 512], BF16)
        mean = k.sb(st, "mgmean", [128, 512], F32)
        var = k.sb(st, "mgvar", [128, 512], F32)
        gt = [k.sb(st, "mggt%d" % i, [128, 3, 512], BF16) for i in range(2)]
        acc = k.sb(st, "mgacc", [128, 512], F32)
        tmp = k.sb(st, "mgtmp", [128, 512], F32)
        mo = [k.sb(st, "mgmo%d" % i, [128, 512], BF16) for i in range(2)]
        pstat = [k.ps(st, "mgpst%d" % i, [128, 512]) for i in range(2)]
        pabc = [k.ps(st, "mgpabc%d" % i, [128, 512]) for i in range(6)]
        ci = 0
        for bi_, wsrc in enumerate((g.w_a, g.w_b, g.w_c)):
            for kk in range(8):
                sg = stg[ci % 2]
                k.dma(None, sg[:], wsrc[l][kk * 128:(kk + 1) * 128, :], writes=[("mgstg", ci % 2)])
                k.op("pool", lambda: nc.gpsimd.tensor_copy(out=wb[bi_][:, kk, :], in_=sg[:]), reads=[("mgstg", ci % 2)], writes=["mgw"])
                ci += 1
        it = 0
        for bi, (c0, w) in enumerate(BLOCKS):
            a_, c_ = ya[0], yc[0]
            ka, kc = ("mgya", 0), ("mgyc", 0)
            k.dma(None, a_[:, :, :w], g.yT[0:1024, :].rearrange("(k p) t -> p k t", p=128)[:, :, c0:c0 + w], reads=[("dram", "yT")], writes=[ka])
            k.dma(None, c_[:, :, :w], g.yT[2048:3072, :].rearrange("(k p) t -> p k t", p=128)[:, :, c0:c0 + w], reads=[("dram", "yT")], writes=[kc])
            k.dma(None, yb32[:, :, :w], fm(g.ybT, c0, w), reads=[("dram", "ybT")], writes=["mgyb32"])
            k.op("act", lambda: nc.scalar.copy(out=ybs[:, :, :w], in_=yb32[:, :, :w]), reads=["mgyb32"], writes=["mgybs"])
            for kk in range(8):
                k.op("pe", lambda: nc.tensor.matmul(pstat[0][:, :w], lhsT=g.ones_bf[:], rhs=ybs[:, kk, :w], start=(kk == 0), stop=(kk == 7)), reads=["mgybs", "ones"], writes=["mgpst0"])
            k.op("dve", lambda: nc.vector.tensor_scalar(out=mean[:, :w], in0=pstat[0][:, :w], scalar1=1.0 / 1024, scalar2=None, op0=ALU.mult), reads=["mgpst0"], writes=["mgmean"])
            k.op("dve", lambda: nc.vector.tensor_tensor(out=yb32[:, :, :w], in0=yb32[:, :, :w], in1=mean[:, :w].unsqueeze(1).to_broadcast([128, 8, w]), op=ALU.subtract),
                 reads=["mgyb32", "mgmean"], writes=["mgyb32"])
            k.op("act", lambda: nc.scalar.activation(out=ybs[:, :, :w], in_=yb32[:, :, :w], func=AF.Square), reads=["mgyb32"], writes=["mgybs"])
            for kk in range(8):
                k.op("pe", lambda: nc.tensor.matmul(pstat[1][:, :w], lhsT=g.ones_bf[:], rhs=ybs[:, kk, :w], start=(kk == 0), stop=(kk == 7)), reads=["mgybs", "ones"], writes=["mgpst1"])
            k.op("dve", lambda: nc.vector.tensor_scalar(out=var[:, :w], in0=pstat[1][:, :w], scalar1=1.0 / 1024, scalar2=EPS, op0=ALU.mult, op1=ALU.add), reads=["mgpst1"], writes=["mgvar"])
            k.op("dve", lambda: nc.vector.reciprocal(out=var[:, :w], in_=var[:, :w]), reads=["mgvar"], writes=["mgvar"])
            k.op("act", lambda: nc.scalar.activation(out=var[:, :w], in_=var[:, :w], func=AF.Sqrt), reads=["mgvar"], writes=["mgvar"])
            k.op("dve", lambda: nc.vector.tensor_tensor(out=yb32[:, :, :w], in0=yb32[:, :, :w], in1=var[:, :w].unsqueeze(1).to_broadcast([128, 8, w]), op=ALU.mult),
                 reads=["mgyb32", "mgvar"], writes=["mgyb32"])
            for kk in range(8):
                k.op("act", lambda: nc.scalar.activation(out=yb32[:, kk, :w], in_=yb32[:, kk, :w], func=AF.Identity, scale=g.lng[:, l, kk:kk + 1], bias=g.lnb[:, l, kk:kk + 1]),
                     reads=["mgyb32", "cw"], writes=["mgyb32"])
            k.op("act", lambda: nc.scalar.activation(out=yb[:, :, :w], in_=yb32[:, :, :w], func=AF.Silu), reads=["mgyb32"], writes=["mgyb"])
            for dc in range(KD):
                gg = gt[it % 2]
                kg = ("mggt", it % 2)
                o = mo[it % 2]
                ko = ("mgmo", it % 2)
                pa, pb, pc = pabc[(it % 2) * 3:(it % 2) * 3 + 3]
                kp = ("mgpabc", it % 2)
                it += 1
                for gi in range(3):
                    r0 = (68 + 16 * gi + dc) * 128
                    k.dma(None, gg[:, gi, :w], g.pT[r0:r0 + 128, c0:c0 + w], reads=[("dram", "pT")], writes=[kg])
                for (pp, src, ks, wi) in ((pa, a_, ka, 0), (pb, yb, "mgyb", 1), (pc, c_, kc, 2)):
                    for kk in range(8):
                        k.op("pe", lambda: nc.tensor.matmul(pp[:, :w], lhsT=wb[wi][:, kk, dc * 128:(dc + 1) * 128], rhs=src[:, kk, :w], start=(kk == 0), stop=(kk == 7)),
                             reads=[ks, "mgw"], writes=[kp])
                k.op("dve", lambda: nc.vector.tensor_tensor(out=acc[:, :w], in0=pa[:, :w], in1=gg[:, 0, :w], op=ALU.mult), reads=[kp, kg], writes=["mgacc"])
                k.op("dve", lambda: nc.vector.tensor_tensor(out=tmp[:, :w], in0=pb[:, :w], in1=gg[:, 1, :w], op=ALU.mult), reads=[kp, kg], writes=["mgtmp"])
                k.op("pool", lambda: nc.gpsimd.tensor_tensor(out=acc[:, :w], in0=acc[:, :w], in1=tmp[:, :w], op=ALU.add), reads=["mgacc", "mgtmp"], writes=["mgacc"])
                k.op("dve", lambda: nc.vector.tensor_tensor(out=tmp[:, :w], in0=pc[:, :w], in1=gg[:, 2, :w], op=ALU.mult), reads=[kp, kg], writes=["mgtmp"])
                k.op("pool", lambda: nc.gpsimd.tensor_tensor(out=o[:, :w], in0=acc[:, :w], in1=tmp[:, :w], op=ALU.add), reads=["mgacc", "mgtmp"], writes=[ko])
                k.dma(None, g.mT[dc * 128:(dc + 1) * 128, c0:c0 + w], o[:, :w], reads=[ko], writes=[("dram", "mT")])


def phase_wout(g, l, src_x, dst_x):
    k, nc = g.k, g.nc
    with ExitStack() as st:
        wb = k.sb(st, "wow", [128, KD, D], BF16)
        stg = [k.sb(st, "wostg%d" % i, [128, D], F32) for i in range(2)]
        mb = [k.sb(st, "wom%d" % i, [128, KD, 512], BF16) for i in range(2)]
        xb = [k.sb(st, "wox%d" % i, [128, KD, 512], F32) for i in range(2)]
        ps = [k.ps(st, "wops%d" % i, [128, 512]) for i in range(4)]
        for kk in range(KD):
            sg = stg[kk % 2]
            k.dma(None, sg[:], g.w_out[l][kk * 128:(kk + 1) * 128, :], writes=[("wostg", kk % 2)])
            k.op("pool", lambda: nc.gpsimd.tensor_copy(out=wb[:, kk, :], in_=sg[:]), reads=[("wostg", kk % 2)], writes=["wow"])
        pi = 0
        for bi, (c0, w) in enumerate(BLOCKS):
            s = 1 if c0 < TC else 0
            m_, x_ = mb[bi % 2], xb[bi % 2]
            km, kx = ("wom", bi % 2), ("wox", bi % 2)
            k.dma(None, m_[:, :, :w], fm(g.mT, c0, w), reads=[("dram", "mT")], writes=[km])
            k.dma(None, x_[:, :, :w], fm(src_x, c0, w), reads=[("dram", src_x.tensor.name)], writes=[kx])
            for dc in range(KD):
                p = ps[pi % 4]
                kp = ("wops", pi % 4)
                pi += 1
                for kk in range(KD):
                    k.op("pe", lambda: nc.tensor.matmul(p[:, :w], lhsT=wb[:, kk, dc * 128:(dc + 1) * 128], rhs=m_[:, kk, :w], start=(kk == 0), stop=(kk == KD - 1)),
                         reads=[km, "wow"], writes=[kp])
                k.op("dve", lambda: nc.vector.scalar_tensor_tensor(out=x_[:, dc, :w], in0=p[:, :w], scalar=g.mods[:, l, 2, dc, s:s + 1], in1=x_[:, dc, :w], op0=ALU.mult, op1=ALU.add),
                     reads=[kp, kx, "mods"], writes=[kx])
            k.dma(None, fm(dst_x, c0, w), x_[:, :, :w], reads=[kx], writes=[("dram", dst_x.tensor.name)])


def phase_norm2_router(g, l):
    k, nc = g.k, g.nc
    R = Ctx()

    def alloc(st):
        R.wr = k.sb(st, "rtw", [128, KD, NE], F32)
        R.ex = k.sb(st, "rtex", [NE, 512], F32)
        R.ri = k.sb(st, "rtri", [NE, 512], F32)
        R.af = k.sb(st, "rtaf", [NE, 512], F32)
        R.ones16 = k.sb(st, "rtones", [NE, NE], F32)
        R.hbf = k.sb(st, "rthbf", [128, KD, 512], BF16)
        R.htok = [k.sb(st, "rthtok%d" % i, [128, D], BF16) for i in range(2)]
        R.psr = k.ps(st, "rtpsr", [NE, 512])
        R.pss = k.ps(st, "rtpss", [NE, 512])
        R.ptt = k.ps(st, "rtptt", [128, KD, 128], BF16)
        R.ti = 0
        k.dma("sp", R.wr[:], g.w_router[l].rearrange("(k p) e -> p k e", p=128), writes=["rtw"])
        k.op("dve", lambda: nc.vector.memset(R.ones16[:], 1.0), writes=["rtones"])

    def extra(bi, c0, w, ho, kh):
        for kk in range(KD):
            k.op("pe", lambda: nc.tensor.matmul(R.psr[:, :w], lhsT=R.wr[:, kk, :], rhs=ho[:, kk, :w], start=(kk == 0), stop=(kk == KD - 1)),
                 reads=[kh, "rtw"], writes=["rtpsr"])
        k.op("act", lambda: nc.scalar.activation(out=R.ex[:, :w], in_=R.psr[:, :w], func=AF.Exp), reads=["rtpsr"], writes=["rtex"])
        k.op("pe", lambda: nc.tensor.matmul(R.pss[:, :w], lhsT=R.ones16[:], rhs=R.ex[:, :w], start=True, stop=True), reads=["rtex", "rtones"], writes=["rtpss"])
        k.op("dve", lambda: nc.vector.reciprocal(out=R.ri[:, :w], in_=R.pss[:, :w]), reads=["rtpss"], writes=["rtri"])
        k.op("dve", lambda: nc.vector.tensor_tensor(out=R.af[:, :w], in0=R.ex[:, :w], in1=R.ri[:, :w], op=ALU.mult), reads=["rtex", "rtri"], writes=["rtaf"])
        k.dma(None, g.affd[:, c0:c0 + w], R.af[:, :w], reads=["rtaf"], writes=[("dram", "affd")])
        k.op("pool", lambda: nc.gpsimd.tensor_copy(out=R.hbf[:, :, :w], in_=ho[:, :, :w]), reads=[kh], writes=["rthbf"])
        for tt in range(w // 128):
            for kk in range(KD):
                k.op("pe", lambda: nc.tensor.transpose(out=R.ptt[:, kk, :], in_=R.hbf[:, kk, tt * 128:(tt + 1) * 128], identity=g.ident_bf[:]),
                     reads=["rthbf", "ident"], writes=["rtptt"])
            ht = R.htok[R.ti % 2]
            kt = ("rthtok", R.ti % 2)
            R.ti += 1
            k.op("dve", lambda: nc.vector.tensor_copy(out=ht[:], in_=R.ptt[:].rearrange("p k f -> p (k f)")), reads=["rtptt"], writes=[kt])
            r0 = c0 + tt * 128
            k.dma(None, g.h2tok[r0:r0 + 128, :], ht[:], reads=[kt], writes=[("dram", "h2tok")])

    phase_norm(g, g.xT, None, F32, lambda kk, s: g.gs2[:, l, kk, s:s + 1], lambda kk, s: g.mods[:, l, 3, kk, s:s + 1], "n2",
               extra=extra, extra_alloc=alloc)


NBIS = 30
BIGI = float(1 << 22)


def phase_route(g, l):
    k, nc = g.k, g.nc
    with ExitStack() as st:
        al = k.sb(st, "ral", [128, NE, 64], F32)
        ac = k.sb(st, "rac", [128, NE, 2], F32)
        cml = k.sb(st, "rcml", [128, NE, 64], F32)
        cmc = k.sb(st, "rcmc", [128, NE, 2], F32)
        lo = k.sb(st, "rlo", [128, 32], F32)
        tcand = k.sb(st, "rtc", [128, 32], F32)
        cnt = k.sb(st, "rcnt", [128, 32], BF16)
        ge = k.sb(st, "rge", [128, 32], F32)
        kc = k.sb(st, "rkc", [128, 32], F32)
        rml = k.sb(st, "rrml", [128, NE, 64], F32)
        rmc = k.sb(st, "rrmc", [128, NE, 2], F32)
        csl = k.sb(st, "rcsl", [128, NE, 64], F32)
        csc = k.sb(st, "rcsc", [128, NE, 2], F32)
        tot = k.sb(st, "rtot", [128, 32], BF16)
        off = k.sb(st, "roff", [128, 32], F32)
        lstr = k.sb(st, "rlstr", [128, 128], BF16)
        gl = k.sb(st, "rgl", [128, NE, 64], F32)
        gc = k.sb(st, "rgc", [128, NE, 2], F32)
        sl = k.sb(st, "rsl", [128, NE, 64], F32)
        sc = k.sb(st, "rsc", [128, NE, 2], F32)
        gli = k.sb(st, "rgli", [128, NE, 64], I32)
        gci = k.sb(st, "rgci", [128, NE, 2], I32)
        sli = k.sb(st, "rsli", [128, NE, 64], I32)
        sci = k.sb(st, "rsci", [128, NE, 2], I32)
        ht = [k.sb(st, "rht%d" % i, [128, D], BF16) for i in range(3)]
        ptot = k.ps(st, "rptot", [128, 32])
        poff = k.ps(st, "rpoff", [128, 32])
        k.dma("sp", al[:], g.affd[:, TC:].rearrange("e (p f) -> p e f", f=64), reads=[("dram", "affd")], writes=["ral"])
        k.dma("act", ac[:], g.affd[:, 0:TC].rearrange("e (p f) -> p e f", f=2), reads=[("dram", "affd")], writes=["rac"])
        k.op("dve", lambda: nc.vector.memset(lo[:], 0.0), writes=["rlo"])
        k.op("dve", lambda: nc.vector.memset(kc[:, 0:16], float(CAPL)), writes=["rkc"])
        k.op("dve", lambda: nc.vector.memset(kc[:, 16:32], float(CAPC)), writes=["rkc"])
        k.op("dve", lambda: nc.vector.memset(rml[:], 1.0), writes=["rrm"])
        k.op("dve", lambda: nc.vector.memset(rml[:, :, 0:1], 0.0), writes=["rrm"])
        k.op("dve", lambda: nc.vector.memset(rmc[:], 1.0), writes=["rrm"])
        k.op("dve", lambda: nc.vector.memset(rmc[:, :, 0:1], 0.0), writes=["rrm"])
        k.op("dve", lambda: nc.vector.tensor_tensor(out=lstr[:], in0=g.cst[:, 128:256], in1=g.cst[:, 0:128], op=ALU.subtract), reads=["cst"], writes=["rlstr"])

        def masks(thr, kthr):
            k.op("dve", lambda: nc.vector.tensor_tensor(out=cml[:], in0=al[:], in1=thr[:, 0:16].unsqueeze(2).to_broadcast([128, NE, 64]), op=ALU.is_ge),
                 reads=["ral", kthr], writes=["rcml"])
            k.op("dve", lambda: nc.vector.tensor_tensor(out=cmc[:], in0=ac[:], in1=thr[:, 16:32].unsqueeze(2).to_broadcast([128, NE, 2]), op=ALU.is_ge),
                 reads=["rac", kthr], writes=["rcmc"])

        lowp = st.enter_context(nc.allow_low_precision("per-partition counts <= 64 are exact in bf16"))
        for it in range(NBIS):
            step = 2.0 ** (-(it + 1))
            k.op("dve", lambda: nc.vector.tensor_scalar(out=tcand[:], in0=lo[:], scalar1=step, scalar2=None, op0=ALU.add), reads=["rlo"], writes=["rtc"])
            masks(tcand, "rtc")
            k.op("dve", lambda: nc.vector.tensor_reduce(out=cnt[:, 0:16], in_=cml[:], axis=AX.X, op=ALU.add), reads=["rcml"], writes=["rcnt"])
            k.op("dve", lambda: nc.vector.tensor_reduce(out=cnt[:, 16:32], in_=cmc[:], axis=AX.X, op=ALU.add), reads=["rcmc"], writes=["rcnt"])
            k.op("pe", lambda: nc.tensor.matmul(ptot[:], lhsT=g.ones_bf[:], rhs=cnt[:], start=True, stop=True), reads=["rcnt", "ones"], writes=["rptot"])
            k.op("dve", lambda: nc.vector.tensor_tensor(out=ge[:], in0=ptot[:], in1=kc[:], op=ALU.is_ge), reads=["rptot", "rkc"], writes=["rge"])
            k.op("dve", lambda: nc.vector.scalar_tensor_tensor(out=lo[:], in0=ge[:], scalar=step, in1=lo[:], op0=ALU.mult, op1=ALU.add), reads=["rge", "rlo"], writes=["rlo"])
        masks(lo, "rlo")
        k.op("dve", lambda: nc.vector.tensor_tensor_scan(out=csl[:].rearrange("p e f -> p (e f)"), data0=rml[:].rearrange("p e f -> p (e f)"),
                                                         data1=cml[:].rearrange("p e f -> p (e f)"), initial=0.0, op0=ALU.mult, op1=ALU.add),
             reads=["rcml", "rrm"], writes=["rcsl"])
        k.op("dve", lambda: nc.vector.tensor_tensor_scan(out=csc[:].rearrange("p e f -> p (e f)"), data0=rmc[:].rearrange("p e f -> p (e f)"),
                                                         data1=cmc[:].rearrange("p e f -> p (e f)"), initial=0.0, op0=ALU.mult, op1=ALU.add),
             reads=["rcmc", "rrm"], writes=["rcsc"])
        k.op("dve", lambda: nc.vector.tensor_copy(out=tot[:, 0:16], in_=csl[:, :, 63]), reads=["rcsl"], writes=["rtot"])
        k.op("dve", lambda: nc.vector.tensor_copy(out=tot[:, 16:32], in_=csc[:, :, 1]), reads=["rcsc"], writes=["rtot"])
        k.op("pe", lambda: nc.tensor.matmul(poff[:], lhsT=lstr[:], rhs=tot[:], start=True, stop=True), reads=["rtot", "rlstr"], writes=["rpoff"])
        k.op("dve", lambda: nc.vector.tensor_copy(out=off[:], in_=poff[:]), reads=["rpoff"], writes=["roff"])
        for (cs_, cm_, g_, s_, gi_, si_, o0, F_, cap, base) in ((csl, cml, gl, sl, gli, sli, 0, 64, CAPL, 0), (csc, cmc, gc, sc, gci, sci, 16, 2, CAPC, CAPL)):
            k.op("dve", lambda: nc.vector.tensor_tensor(out=cs_[:], in0=cs_[:], in1=off[:, o0:o0 + 16].unsqueeze(2).to_broadcast([128, NE, F_]), op=ALU.add),
                 reads=["rcsl", "rcsc", "roff"], writes=["rcsl", "rcsc"])
            k.op("dve", lambda: nc.vector.tensor_scalar(out=g_[:], in0=cs_[:], scalar1=float(cap), scalar2=None, op0=ALU.is_le), reads=["rcsl", "rcsc"], writes=["rg"])
            k.op("dve", lambda: nc.vector.tensor_tensor(out=cm_[:], in0=cm_[:], in1=g_[:], op=ALU.mult), reads=["rg", "rcml", "rcmc"], writes=["rcml", "rcmc"])
            k.op("dve", lambda: nc.vector.tensor_scalar(out=g_[:], in0=cs_[:], scalar1=float(base - 1 - SLOTS), scalar2=None, op0=ALU.add), reads=["rcsl", "rcsc"], writes=["rg"])
            k.op("dve", lambda: nc.vector.tensor_tensor(out=g_[:], in0=g_[:], in1=cm_[:], op=ALU.mult), reads=["rg", "rcml", "rcmc"], writes=["rg"])
            k.op("dve", lambda: nc.vector.tensor_tensor(out=g_[:], in0=g_[:], in1=g.ebase[:].unsqueeze(2).to_broadcast([128, NE, F_]), op=ALU.add), reads=["rg", "ebase"], writes=["rg"])
            k.op("dve", lambda: nc.vector.tensor_scalar(out=s_[:], in0=cm_[:], scalar1=-BIGI, scalar2=BIGI, op0=ALU.mult, op1=ALU.add), reads=["rcml", "rcmc"], writes=["rs"])
            k.op("dve", lambda: nc.vector.tensor_tensor(out=s_[:], in0=s_[:], in1=g_[:], op=ALU.add), reads=["rs", "rg"], writes=["rs"])
            k.op("dve", lambda: nc.vector.tensor_copy(out=gi_[:], in_=g_[:]), reads=["rg"], writes=["rgi"])
            k.op("dve", lambda: nc.vector.tensor_copy(out=si_[:], in_=s_[:]), reads=["rs"], writes=["rsi"])
        k.dma("sp", g.gix[:, TC:].rearrange("e (p f) -> p e f", f=64), gli[:], reads=["rgi"], writes=[("dram", "gix")])
        k.dma("act", g.gix[:, 0:TC].rearrange("e (p f) -> p e f", f=2), gci[:], reads=["rgi"], writes=[("dram", "gix")])
        breg = nc.gpsimd.to_reg(NE * SROWS - 1)
        hi = 0
        for (F_, si_, rbase) in ((64, sli, TC), (2, sci, 0)):
            for f in range(F_):
                h_ = ht[hi % 3]
                kh = ("rht", hi % 3)
                hi += 1
                k.dma(None, h_[:], g.h2tok[rbase:rbase + 128 * F_, :].rearrange("(p f) d -> p f d", f=F_)[:, f, :], reads=[("dram", "h2tok")], writes=[kh])
                for e in range(NE):
                    k.dma("pool", g.xsd, h_[:], reads=[kh, "rsi"], writes=[("dram", "xsd")],
                          indirect=dict(out_offset=bass.IndirectOffsetOnAxis(ap=si_[:, e, f:f + 1], axis=0), in_offset=None,
                                        bounds_check=breg, oob_is_err=False))


SBLK = [(0, 512), (512, 512), (1024, 32)]


def phase_experts(g, l, experts=range(NE)):
    k, nc = g.k, g.nc
    NT = (SLOTS + 127) // 128
    with ExitStack() as st:
        wg = k.sb(st, "exwg", [128, KD, DFF], BF16)
        wu = k.sb(st, "exwu", [128, KD, DFF], BF16)
        wd = k.sb(st, "exwd", [128, 8, D], BF16)
        stg = [k.sb(st, "exstg%d" % i, [128, D], F32) for i in range(2)]
        xt = [k.sb(st, "exxt%d" % i, [128, D], BF16) for i in range(2)]
        xsT = k.sb(st, "exxsT", [128, KD, SLOTS], BF16)
        hid = k.sb(st, "exhid", [128, 8, SLOTS], BF16)
        sgt = [k.sb(st, "exsg%d" % i, [128, 512], BF16) for i in range(2)]
        yt = [k.sb(st, "exyt%d" % i, [128, D], BF16) for i in range(2)]
        ptx = [k.ps(st, "exptx%d" % i, [128, 4, 128], BF16) for i in range(2)]
        pg = [k.ps(st, "expg%d" % i, [128, 512]) for i in range(2)]
        pu = [k.ps(st, "expu%d" % i, [128, 512]) for i in range(2)]
        pd = [k.ps(st, "expd%d" % i, [128, 512]) for i in range(2)]
        ci = 0
        xi = 0
        pi = 0
        qi = 0
        yi = 0
        casters = ("pool", "act", "dve")

        def cast(dst, src, rk, wk):
            nonlocal ci
            e_ = casters[ci % 3]
            if e_ == "pool":
                k.op("pool", lambda: nc.gpsimd.tensor_copy(out=dst, in_=src), reads=[rk], writes=[wk])
            elif e_ == "act":
                k.op("act", lambda: nc.scalar.copy(out=dst, in_=src), reads=[rk], writes=[wk])
            else:
                k.op("dve", lambda: nc.vector.tensor_copy(out=dst, in_=src), reads=[rk], writes=[wk])

        for e in experts:
            for (wsb, wsrc, nk, wk) in ((wg, g.w_eg, KD, "exwg"), (wu, g.w_eu, KD, "exwu")):
                for k2 in range(0, nk, 2):
                    sg = stg[ci % 2]
                    ks = ("exstg", ci % 2)
                    k.dma(None, sg[:].rearrange("p (a c) -> p a c", a=2), wsrc[l, e][k2 * 128:(k2 + 2) * 128, :].rearrange("(a p) c -> p a c", p=128), writes=[ks])
                    cast(wsb[:, k2:k2 + 2, :], sg[:].rearrange("p (a c) -> p a c", a=2), ks, wk)
                    ci += 1
            for k2 in range(8):
                sg = stg[ci % 2]
                ks = ("exstg", ci % 2)
                k.dma(None, sg[:], g.w_ed[l, e][k2 * 128:(k2 + 1) * 128, :], writes=[ks])
                cast(wd[:, k2, :], sg[:], ks, "exwd")
                ci += 1
            for j in range(NT):
                rows = min(128, SLOTS - j * 128)
                x_ = xt[xi % 2]
                kx = ("exxt", xi % 2)
                xi += 1
                r0 = e * SROWS + j * 128
                k.dma(None, x_[:rows, :], g.xsd[r0:r0 + rows, :], reads=[("dram", "xsd")], writes=[kx])
                for k4 in range(0, KD, 4):
                    p_ = ptx[pi % 2]
                    kp = ("exptx", pi % 2)
                    pi += 1
                    for a in range(4):
                        kk = k4 + a
                        k.op("pe", lambda: nc.tensor.transpose(out=p_[:, a, :rows], in_=x_[:rows, kk * 128:(kk + 1) * 128], identity=g.ident_bf[:rows, :rows]),
                             reads=[kx, "ident"], writes=[kp])
                    k.op("dve" if (pi % 2) else "act",
                         (lambda: nc.vector.tensor_copy(out=xsT[:, k4:k4 + 4, j * 128:j * 128 + rows], in_=p_[:, :, :rows])) if (pi % 2) else
                         (lambda: nc.scalar.copy(out=xsT[:, k4:k4 + 4, j * 128:j * 128 + rows], in_=p_[:, :, :rows])),
                         reads=[kp], writes=["exxsT"])
            for fc in range(8):
                for (s0, sw) in SBLK:
                    g_, u_ = pg[qi % 2], pu[qi % 2]
                    kg_, ku_ = ("expg", qi % 2), ("expu", qi % 2)
                    sg_ = sgt[qi % 2]
                    ksg = ("exsg", qi % 2)
                    qi += 1
                    for kk in range(KD):
                        k.op("pe", lambda: nc.tensor.matmul(g_[:, :sw], lhsT=wg[:, kk, fc * 128:(fc + 1) * 128], rhs=xsT[:, kk, s0:s0 + sw], start=(kk == 0), stop=(kk == KD - 1)),
                             reads=["exwg", "exxsT"], writes=[kg_])
                    for kk in range(KD):
                        k.op("pe", lambda: nc.tensor.matmul(u_[:, :sw], lhsT=wu[:, kk, fc * 128:(fc + 1) * 128], rhs=xsT[:, kk, s0:s0 + sw], start=(kk == 0), stop=(kk == KD - 1)),
                             reads=["exwu", "exxsT"], writes=[ku_])
                    k.op("act", lambda: nc.scalar.activation(out=sg_[:, :sw], in_=g_[:, :sw], func=AF.Silu), reads=[kg_], writes=[ksg])
                    k.op("dve", lambda: nc.vector.tensor_tensor(out=hid[:, fc, s0:s0 + sw], in0=u_[:, :sw], in1=sg_[:, :sw], op=ALU.mult), reads=[ku_, ksg], writes=["exhid"])
            for j in range(NT):
                rows = min(128, SLOTS - j * 128)
                y_ = yt[yi % 2]
                ky = ("exyt", yi % 2)
                yi += 1
                for db in range(4):
                    d_ = pd[(yi * 4 + db) % 2]
                    kd_ = ("expd", (yi * 4 + db) % 2)
                    for fc in range(8):
                        k.op("pe", lambda: nc.tensor.matmul(d_[:rows, :], lhsT=hid[:, fc, j * 128:j * 128 + rows], rhs=wd[:, fc, db * 512:(db + 1) * 512], start=(fc == 0), stop=(fc == 7)),
                             reads=["exhid", "exwd"], writes=[kd_])
                    if db % 2 == 0:
                        k.op("act", lambda: nc.scalar.copy(out=y_[:rows, db * 512:(db + 1) * 512], in_=d_[:rows, :]), reads=[kd_], writes=[ky])
                    else:
                        k.op("dve", lambda: nc.vector.tensor_copy(out=y_[:rows, db * 512:(db + 1) * 512], in_=d_[:rows, :]), reads=[kd_], writes=[ky])
                r0 = e * SROWS + j * 128
                k.dma(None, g.ysd[r0:r0 + rows, :], y_[:rows, :], reads=[ky], writes=[("dram", "ysd")])


def phase_combine(g, l):
    k, nc = g.k, g.nc
    with ExitStack() as st:
        gi = [k.sb(st, "cbgi%d" % i, [128, NE], I32) for i in range(2)]
        aw = [k.sb(st, "cbaw%d" % i, [128, NE], F32) for i in range(2)]
        dg = [k.sb(st, "cbdg%d" % i, [128, NE, 128], BF16) for i in range(2)]
        G = [k.sb(st, "cbG%d" % i, [128, D], BF16) for i in range(NE)]
        xb = [k.sb(st, "cbx%d" % i, [128, KD, 128], F32) for i in range(2)]
        pacc = [k.ps(st, "cbacc%d" % i, [128, 4, 128]) for i in range(4)]
        gq = 0
        for ti in range(T // 128):
            c0 = ti * 128
            s = 1 if c0 < TC else 0
            gi_, aw_, dg_, x_ = gi[ti % 2], aw[ti % 2], dg[ti % 2], xb[ti % 2]
            kgi, kaw, kdg, kx = ("cbgi", ti % 2), ("cbaw", ti % 2), ("cbdg", ti % 2), ("cbx", ti % 2)
            k.dma("sp", gi_[:], g.gix[:, c0:c0 + 128].rearrange("e t -> t e"), reads=[("dram", "gix")], writes=[kgi], allow_slow_non_contiguous=True)
            k.dma("act", aw_[:], g.affd[:, c0:c0 + 128].rearrange("e t -> t e"), reads=[("dram", "affd")], writes=[kaw], allow_slow_non_contiguous=True)
            k.dma(None, x_[:], fm(g.xT, c0, 128), reads=[("dram", "xT")], writes=[kx])
            for e in range(NE):
                k.op("dve", lambda: nc.vector.tensor_scalar(out=dg_[:, e, :], in0=g.ident_bf[:], scalar1=aw_[:, e:e + 1], scalar2=None, op0=ALU.mult),
                     reads=["ident", kaw], writes=[kdg])
            for e in range(NE):
                k.dma("pool", G[e][:], g.ysd, reads=[kgi, ("dram", "ysd")], writes=[("cbG", e)],
                      indirect=dict(out_offset=None, in_offset=bass.IndirectOffsetOnAxis(ap=gi_[:, e:e + 1], axis=0)))
            for kk in range(KD):
                for e in range(NE):
                    k.op("pe", lambda: nc.tensor.matmul(pacc[kk // 4][:, kk % 4, :], lhsT=G[e][:, kk * 128:(kk + 1) * 128], rhs=dg_[:, e, :], start=(e == 0), stop=(e == NE - 1)),
                         reads=[("cbG", e), kdg], writes=["cbacc"])
            for kk in range(KD):
                k.op("dve", lambda: nc.vector.scalar_tensor_tensor(out=x_[:, kk, :], in0=pacc[kk // 4][:, kk % 4, :], scalar=g.mods[:, l, 5, kk, s:s + 1], in1=x_[:, kk, :],
                                                                   op0=ALU.mult, op1=ALU.add), reads=["cbacc", kx, "mods"], writes=[kx])
            k.dma(None, fm(g.xT, c0, 128), x_[:], reads=[kx], writes=[("dram", "xT")])


_PROG = None


def kernel(**inputs):
    global _PROG
    if _PROG is None:
        _PROG = make_program()
    nc, _ = _PROG
    m = host_inputs(inputs)
    res = run_bass_kernel_spmd(nc, [m], core_ids=[0])
    outT = np.asarray(res.results[0]["outT"], np.float32)
    return np.ascontiguousarray(outT.T)[None]
```

```python
import numpy as np
import concourse.bass as bass
import concourse.mybir as mybir
from concourse.bass_utils import run_bass_kernel_spmd
from contextlib import ExitStack

F32 = mybir.dt.float32
BF16 = mybir.dt.bfloat16
I32 = mybir.dt.int32
AF = mybir.ActivationFunctionType
ALU = mybir.AluOpType
AX = mybir.AxisListType

D = 2048
KD = 16
TC = 256
TL = 8192
T = TC + TL
DEPTH = 4
EPS = 1e-6
NE = 16
DFF = 1024
CAPL = 1024
CAPC = 32
SLOTS = CAPL + CAPC
SROWS = SLOTS + 1
BLOCKS = [(0, TC)] + [(TC + 512 * i, 512) for i in range(TL // 512)]
DIN = 14848


class K:
    ROT = 60000

    def __init__(self, nc, n_dma_sems=48):
        self.nc = nc
        self.es = ExitStack()
        self.engs = {"pe": nc.tensor, "dve": nc.vector, "act": nc.scalar, "pool": nc.gpsimd, "sp": nc.sync}
        self.sem = {}
        self.cnt = {}
        self.nsem = 0
        for e in self.engs:
            self.sem[e] = self._newsem(e)
            self.cnt[e] = 0
        self.dma_sems = [self._newsem("dma%d" % i) for i in range(n_dma_sems)]
        self.dma_val = [0] * n_dma_sems
        self.dma_rr = 0
        self.seen = {e: {} for e in self.engs}
        self.lastw = {}
        self.readers = {}
        self.ninst = 0
        self.qrr = 0

    def _newsem(self, name):
        self.nsem += 1
        return self.es.enter_context(self.nc.semaphore("s_%s_%d" % (name, self.nsem)))

    def _wait(self, e, dep):
        s, v = dep
        sid = id(s)
        if self.seen[e].get(sid, 0) >= v:
            return
        self.engs[e].wait_ge(s, v)
        self.seen[e][sid] = v

    def _deps(self, e, reads, writes):
        for k in reads:
            d = self.lastw.get(k)
            if d is not None and not (e == "pe" and d[2] == "pe"):
                self._wait(e, (d[0], d[1]))
        for k in writes:
            d = self.lastw.get(k)
            if d is not None and not (e == "pe" and d[2] == "pe"):
                self._wait(e, (d[0], d[1]))
            for d in self.readers.get(k, ()):
                if not (e == "pe" and d[2] == "pe"):
                    self._wait(e, (d[0], d[1]))

    def _record(self, dep, reads, writes):
        for k in writes:
            self.lastw[k] = dep
            self.readers[k] = []
        for k in reads:
            self.readers.setdefault(k, []).append(dep)

    def op(self, e, fn, reads=(), writes=()):
        self._deps(e, reads, writes)
        inst = fn()
        if self.cnt[e] >= self.ROT:
            self.sem[e] = self._newsem(e)
            self.cnt[e] = 0
        self.cnt[e] += 1
        inst.then_inc(self.sem[e], 1)
        self._record((self.sem[e], self.cnt[e], e), reads, writes)
        self.ninst += 1
        return inst

    def dma(self, q, out, in_, reads=(), writes=(), indirect=None, **kw):
        if q is None:
            q = ("sp", "act")[self.qrr % 2]
            self.qrr += 1
        self._deps(q, reads, writes)
        i = self.dma_rr
        self.dma_rr = (self.dma_rr + 1) % len(self.dma_sems)
        s = self.dma_sems[i]
        if self.dma_val[i] > 0:
            self._wait(q, (s, self.dma_val[i]))
        if indirect is None:
            inst = self.engs[q].dma_start(out=out, in_=in_, **kw)
        else:
            inst = self.engs[q].indirect_dma_start(out=out, in_=in_, **indirect)
        self.dma_val[i] += 16
        inst.then_inc(s, 16)
        self._record((s, self.dma_val[i], "dma"), reads, writes)
        self.ninst += 1
        return inst

    def finish(self, keys):
        for k in keys:
            d = self.lastw.get(k)
            if d is not None:
                self._wait("sp", (d[0], d[1]))

    def sb(self, st, name, shape, dt):
        self.uid = getattr(self, "uid", 0) + 1
        return st.enter_context(self.nc.sbuf_tensor("%s_%d" % (name, self.uid), list(shape), dt))

    def ps(self, st, name, shape, dt=F32):
        self.uid = getattr(self, "uid", 0) + 1
        return st.enter_context(self.nc.psum_tensor("%s_%d" % (name, self.uid), list(shape), dt))


class Ctx:
    pass


def fm(ap, c0, w):
    return ap.rearrange("(k p) t -> p k t", p=128)[:, :, c0:c0 + w]


def phase_mods(g, layers):
    k, nc = g.k, g.nc
    with ExitStack() as st:
        cc = k.sb(st, "cc", [128, KD, 2], F32)
        sc = k.sb(st, "sc", [128, KD, 2], F32)
        bada = k.sb(st, "bada", [128, DEPTH, 6, KD], F32)
        wst = [k.sb(st, "wada%d" % i, [128, KD, 512], F32) for i in range(2)]
        ps = k.ps(st, "psm", [128, 512])
        k.dma("sp", cc[:], g.cvec, writes=["cc"])
        k.dma("act", bada[:], g.b_ada, writes=["bada"])
        k.op("act", lambda: nc.scalar.activation(out=sc[:], in_=cc[:], func=AF.Silu), reads=["cc"], writes=["sc"])
        it = 0
        for l in layers:
            for j in range(6):
                for q in range(4):
                    w = wst[it % 2]
                    wk = ("wada", it % 2)
                    col0 = j * D + q * 512
                    k.dma(None, w[:], g.w_ada[l].rearrange("(k p) c -> p k c", p=128)[:, :, col0:col0 + 512],
                          writes=[wk])
                    for m in range(4):
                        for kk in range(KD):
                            k.op("pe", lambda: nc.tensor.matmul(ps[:, m * 2:m * 2 + 2], lhsT=w[:, kk, m * 128:(m + 1) * 128],
                                                                 rhs=sc[:, kk, :], start=(kk == 0), stop=(kk == KD - 1)),
                                 reads=[wk, "sc"], writes=["psm"])
                    for m in range(4):
                        kc = q * 4 + m
                        k.op("dve", lambda: nc.vector.tensor_scalar(out=g.mods[:, l, j, kc, :], in0=ps[:, m * 2:m * 2 + 2],
                                                                    scalar1=bada[:, l, j, kc:kc + 1], scalar2=None, op0=ALU.add),
                             reads=["psm", "bada"], writes=["mods"])
                    it += 1
        for l in layers:
            for (dst, gsrc, j) in ((g.gs1, g.n1g, 1), (g.gs2, g.n2g, 4)):
                k.op("dve", lambda: nc.vector.tensor_scalar(out=dst[:, l, :, :], in0=g.mods[:, l, j, :, :], scalar1=1.0, scalar2=None, op0=ALU.add),
                     reads=["mods"], writes=["gs"])
                k.op("dve", lambda: nc.vector.tensor_tensor(out=dst[:, l, :, :], in0=dst[:, l, :, :],
                                                            in1=gsrc[:, l, :].unsqueeze(2).to_broadcast([128, KD, 2]), op=ALU.mult),
                     reads=["gs", "ng"], writes=["gs"])


def phase_norm(g, src, dst, dst_dt, gs, sh, tag, blocks=BLOCKS, extra=None, dst_off=0, extra_alloc=None):
    k, nc = g.k, g.nc
    with ExitStack() as st:
        xin = [k.sb(st, "nx%d" % i, [128, KD, 512], F32) for i in range(2)]
        sq = k.sb(st, "nsq", [128, KD, 512], BF16)
        nhb = 1 if dst_dt == F32 else 2
        hout = [k.sb(st, "nh%d" % i, [128, KD, 512], dst_dt) for i in range(nhb)]
        rstd = k.sb(st, "nrstd", [128, 512], F32)
        ps = k.ps(st, "nps", [128, 512])
        if extra_alloc is not None:
            extra_alloc(st)
        for bi, (c0, w) in enumerate(blocks):
            s = 1 if c0 < TC else 0
            xi = xin[bi % 2]
            ho = hout[bi % nhb]
            kx = (tag + "x", bi % 2)
            kh = (tag + "h", bi % nhb)
            k.dma(None, xi[:, :, :w], fm(src, c0, w), reads=[("dram", src.tensor.name)], writes=[kx])
            k.op("act", lambda: nc.scalar.activation(out=sq[:, :, :w], in_=xi[:, :, :w], func=AF.Square), reads=[kx], writes=["nsq"])
            for kk in range(KD):
                k.op("pe", lambda: nc.tensor.matmul(ps[:, :w], lhsT=g.ones_bf[:], rhs=sq[:, kk, :w], start=(kk == 0), stop=(kk == KD - 1)),
                     reads=["nsq"], writes=["nps"])
            k.op("dve", lambda: nc.vector.tensor_scalar(out=rstd[:, :w], in0=ps[:, :w], scalar1=1.0 / D, scalar2=EPS, op0=ALU.mult, op1=ALU.add),
                 reads=["nps"], writes=["nrstd"])
            k.op("dve", lambda: nc.vector.reciprocal(out=rstd[:, :w], in_=rstd[:, :w]), reads=["nrstd"], writes=["nrstd"])
            k.op("act", lambda: nc.scalar.activation(out=rstd[:, :w], in_=rstd[:, :w], func=AF.Sqrt), reads=["nrstd"], writes=["nrstd"])
            k.op("dve", lambda: nc.vector.tensor_tensor(out=xi[:, :, :w], in0=xi[:, :, :w],
                                                        in1=rstd[:, :w].unsqueeze(1).to_broadcast([128, KD, w]), op=ALU.mult),
                 reads=[kx, "nrstd"], writes=[kx])
            for kk in range(KD):
                if sh is not None:
                    k.op("act", lambda: nc.scalar.activation(out=ho[:, kk, :w], in_=xi[:, kk, :w], func=AF.Identity,
                                                             scale=gs(kk, s), bias=sh(kk, s)),
                         reads=[kx, "mods", "gs"], writes=[kh])
                else:
                    k.op("act", lambda: nc.scalar.activation(out=ho[:, kk, :w], in_=xi[:, kk, :w], func=AF.Identity,
                                                             scale=gs(kk, s)),
                         reads=[kx, "mods", "gs"], writes=[kh])
            if dst is not None:
                k.dma(None, fm(dst, c0 - dst_off, w), ho[:, :, :w], reads=[kh], writes=[("dram", dst.tensor.name)])
            if extra is not None:
                extra(bi, c0, w, ho, kh)


def evac_kind(mi):
    if 16 <= mi < 32:
        return "sigf"
    if 32 <= mi < 40:
        return "silu"
    if 48 <= mi < 56 or mi >= 68:
        return "sig"
    return "copy"


def phase_inproj(g, l, mchunks=None):
    k, nc = g.k, g.nc
    if mchunks is None:
        mchunks = list(range(DIN // 128))
    G = 29
    groups = [mchunks[i:i + G] for i in range(0, len(mchunks), G)]
    wsrc = g.w_in[l].rearrange("(k p) c -> p k c", p=128)
    with ExitStack() as st:
        wg = k.sb(st, "ipw", [128, G, KD, 128], BF16)
        stg = [k.sb(st, "ipstg%d" % i, [128, KD, 128], F32) for i in range(2)]
        hb = [k.sb(st, "iph%d" % i, [128, KD, 512], BF16) for i in range(2)]
        ob = [k.sb(st, "ipo%d" % i, [128, 512], BF16) for i in range(4)]
        of = [k.sb(st, "ipf%d" % i, [128, 512], F32) for i in range(2)]
        ps = [k.ps(st, "ipps%d" % i, [128, 512]) for i in range(4)]
        ci = 0
        pi = 0
        oi = 0
        fi = 0
        hi = 0
        for grp in groups:
            for gi, mi in enumerate(grp):
                sg = stg[ci % 2]
                k.dma(None, sg[:], wsrc[:, :, mi * 128:(mi + 1) * 128], writes=[("ipstg", ci % 2)])
                k.op("pool", lambda: nc.gpsimd.tensor_copy(out=wg[:, gi, :, :], in_=sg[:]), reads=[("ipstg", ci % 2)], writes=[("ipw", gi)])
                ci += 1
            for (c0, w) in BLOCKS:
                h = hb[hi % 2]
                kh = ("iph", hi % 2)
                hi += 1
                k.dma(None, h[:, :, :w], fm(g.hT, c0, w), reads=[("dram", "hT")], writes=[kh])
                for gi, mi in enumerate(grp):
                    p = ps[pi % 4]
                    kp = ("ipps", pi % 4)
                    pi += 1
                    for kk in range(KD):
                        k.op("pe", lambda: nc.tensor.matmul(p[:, :w], lhsT=wg[:, gi, kk, :], rhs=h[:, kk, :w], start=(kk == 0), stop=(kk == KD - 1)),
                             reads=[kh, ("ipw", gi)], writes=[kp])
                    kind = evac_kind(mi)
                    if kind == "sigf":
                        o = of[fi % 2]
                        ko = ("ipf", fi % 2)
                        fi += 1
                        k.op("act", lambda: nc.scalar.activation(out=o[:, :w], in_=p[:, :w], func=AF.Sigmoid), reads=[kp], writes=[ko])
                        zr = (mi - 16) * 128
                        k.dma(None, g.zT[zr:zr + 128, c0:c0 + w], o[:, :w], reads=[ko], writes=[("dram", "zT")])
                    else:
                        o = ob[oi % 4]
                        ko = ("ipo", oi % 4)
                        oi += 1
                        if kind == "copy":
                            k.op("dve", lambda: nc.vector.tensor_copy(out=o[:, :w], in_=p[:, :w]), reads=[kp], writes=[ko])
                        else:
                            fn = AF.Silu if kind == "silu" else AF.Sigmoid
                            k.op("act", lambda: nc.scalar.activation(out=o[:, :w], in_=p[:, :w], func=fn), reads=[kp], writes=[ko])
                        k.dma(None, g.pT[mi * 128:(mi + 1) * 128, c0:c0 + w], o[:, :w], reads=[ko], writes=[("dram", "pT")])


def drain(k):
    for q in ("sp", "act", "pool", "pe", "dve"):
        for i, s in enumerate(k.dma_sems):
            if k.dma_val[i] > 0:
                k._wait(q, (s, k.dma_val[i]))
        for e2 in ("pe", "dve", "act", "pool"):
            if e2 != q and k.cnt[e2] > 0:
                k._wait(q, (k.sem[e2], k.cnt[e2]))


def make_program(layers=(0, 1, 2, 3), stages=None, feed=(), dump=()):
    nc = bass.Bass("TRN2", target_bir_lowering=False)
    k = K(nc)
    g = Ctx()
    g.k, g.nc = k, nc
    allst = stages is None
    g.dbg = getattr(make_program, 'dbg', False)

    def on(s):
        return allst or s in stages

    def ext_in(name, shape, dt):
        return nc.dram_tensor(name, list(shape), dt, kind="ExternalInput").ap()

    def scratch(name, shape, dt):
        if name in feed:
            return nc.dram_tensor(name, list(shape), dt, kind="ExternalInput").ap()
        if name in dump:
            return nc.dram_tensor(name, list(shape), dt, kind="ExternalOutput").ap()
        return nc.dram_tensor(name, list(shape), dt).ap()

    g.xT0 = ext_in("xT0", [D, T], F32)
    g.cvec = ext_in("cvec", [128, KD, 2], F32)
    g.b_ada = ext_in("b_ada", [128, DEPTH, 6, KD], F32)
    if on("mods"):
        g.w_ada = ext_in("w_ada", [DEPTH, D, 6 * D], F32)
    g.n1g_d = ext_in("n1g", [128, DEPTH, KD], F32)
    g.n2g_d = ext_in("n2g", [128, DEPTH, KD], F32)
    if on("inproj"):
        g.w_in = ext_in("w_in", [DEPTH, D, DIN], F32)
    g.cst_d = ext_in("cst", [128, 128 * 4], F32)
    g.lbl_d = ext_in("lbl", [128, DEPTH, 16], F32)
    g.ong_d = ext_in("ong", [128, DEPTH, 8], F32)
    g.cw_d = ext_in("cw", [128, DEPTH, 8, 31], F32)
    g.cb_d = ext_in("cb", [128, DEPTH, 8], F32)
    g.sink_d = ext_in("sink", [128, DEPTH, 8], F32)
    g.rope_d = ext_in("rope", [2, 128, TL], F32)
    g.lng_d = ext_in("lng", [128, DEPTH, 8], F32)
    g.lnb_d = ext_in("lnb", [128, DEPTH, 8], F32)
    if on("merge"):
        g.w_a = ext_in("w_a", [DEPTH, 1024, D], F32)
        g.w_b = ext_in("w_b", [DEPTH, 1024, D], F32)
        g.w_c = ext_in("w_c", [DEPTH, 1024, D], F32)
    if on("wout"):
        g.w_out = ext_in("w_out", [DEPTH, D, D], F32)
    g.w_router = ext_in("w_router", [DEPTH, D, NE], F32)
    g.ebase_d = ext_in("ebase", [128, NE], F32)
    g.fng_d = ext_in("fng", [128, KD], F32)
    if on("experts"):
        g.w_eg = ext_in("w_eg", [DEPTH, NE, D, DFF], F32)
        g.w_eu = ext_in("w_eu", [DEPTH, NE, D, DFF], F32)
        g.w_ed = ext_in("w_ed", [DEPTH, NE, DFF, D], F32)
    g.outT = nc.dram_tensor("outT", [D, TL], F32, kind="ExternalOutput").ap()

    g.hT = scratch("hT", [D, T], BF16)
    g.pT = scratch("pT", [DIN, T], BF16)
    g.zT = scratch("zT", [2048, T], F32)
    g.affd = scratch("affd", [NE, T], F32)
    g.gix = scratch("gix", [NE, T], I32)
    g.h2tok = scratch("h2tok", [T, D], BF16)
    g.xsd = scratch("xsd", [NE * SROWS, D], BF16)
    g.ysd = scratch("ysd", [NE * SROWS, D], BF16)
    g.mT = scratch("mT", [D, T], BF16)
    g.xT = scratch("xT", [D, T], F32)
    g.oT = scratch("oT", [1024, T], F32)
    g.yT = scratch("yT", [3072, T], BF16)
    g.ybT = scratch("ybT", [1024, T], F32)
    g.modsd = scratch("modsd", [128, DEPTH * 6 * KD * 2], F32)
    outs = []

    g.gsd = scratch("gsd", [2, 128, DEPTH * KD * 2], F32)
    with ExitStack() as st:
        g.pad0 = k.sb(st, "pad0_s", [128, 2048], F32)
        g.mods = k.sb(st, "mods_s", [128, DEPTH, 6, KD, 2], F32)
        g.gs1 = k.sb(st, "gs1", [128, DEPTH, KD, 2], F32)
        g.gs2 = k.sb(st, "gs2", [128, DEPTH, KD, 2], F32)
        g.n1g = k.sb(st, "n1gs", [128, DEPTH, KD], F32)
        g.n2g = k.sb(st, "n2gs", [128, DEPTH, KD], F32)
        cst = k.sb(st, "cst_s", [128, 128 * 4], F32)
        g.ones_bf = k.sb(st, "ones_bf", [128, 128], BF16)
        g.ident_bf = k.sb(st, "ident_bf", [128, 128], BF16)
        g.lb = k.sb(st, "lb_s", [128, DEPTH, 16], F32)
        g.oml = k.sb(st, "oml_s", [128, DEPTH, 16], F32)
        g.ong = k.sb(st, "ong_s", [128, DEPTH, 8], F32)
        g.cw = k.sb(st, "cw_s", [128, DEPTH, 8, 31], F32)
        g.cb = k.sb(st, "cb_s", [128, DEPTH, 8], F32)
        g.sink = k.sb(st, "sink_s", [128, DEPTH, 8], F32)
        k.dma("sp", g.ong[:], g.ong_d, writes=["ong"])
        k.dma("act", g.cw[:], g.cw_d, writes=["cw"])
        k.dma("sp", g.cb[:], g.cb_d, writes=["cw"])
        k.dma("act", g.sink[:], g.sink_d, writes=["sink"])
        g.ebase = k.sb(st, "ebase_s", [128, NE], F32)
        g.fng = k.sb(st, "fng_s", [128, KD], F32)
        zrow = k.sb(st, "zrow_s", [NE, D], BF16)
        k.dma("sp", g.ebase[:], g.ebase_d, writes=["ebase"])
        k.dma("act", g.fng[:], g.fng_d, writes=["fng"])
        k.op("dve", lambda: nc.vector.memset(zrow[:], 0.0), writes=["zrow"])
        k.dma("sp", g.ysd.rearrange("(e r) d -> e r d", r=SROWS)[:, SLOTS, :], zrow[:], reads=["zrow"], writes=[("dram", "ysd")])
        g.lng = k.sb(st, "lng_s", [128, DEPTH, 8], F32)
        g.lnb = k.sb(st, "lnb_s", [128, DEPTH, 8], F32)
        k.dma("sp", g.lng[:], g.lng_d, writes=["cw"])
        k.dma("act", g.lnb[:], g.lnb_d, writes=["cw"])
        k.dma("sp", g.n1g[:], g.n1g_d, writes=["ng"])
        k.dma("act", g.n2g[:], g.n2g_d, writes=["ng"])
        k.dma("sp", cst[:], g.cst_d, writes=["cst"])
        k.op("dve", lambda: nc.vector.memset(g.ones_bf[:], 1.0), writes=["ones"])
        k.op("dve", lambda: nc.vector.tensor_copy(out=g.ident_bf[:], in_=cst[:, 0:128]), reads=["cst"], writes=["ident"])
        g.cst = cst

        def reload():
            if not on("mods"):
                return
            k.dma("sp", g.mods[:].rearrange("p l j k s -> p (l j k s)"), g.modsd, reads=[("dram", "modsd")], writes=["mods"])
            k.dma("act", g.gs1[:].rearrange("p l k s -> p (l k s)"), g.gsd[0], reads=[("dram", "gsd")], writes=["gs"])
            k.dma("sp", g.gs2[:].rearrange("p l k s -> p (l k s)"), g.gsd[1], reads=[("dram", "gsd")], writes=["gs"])

        if on("mods"):
            phase_mods(g, layers)
            if "modsd" not in dump:
                k.dma("sp", g.modsd, g.mods[:].rearrange("p l j k s -> p (l j k s)"), reads=["mods"], writes=[("dram", "modsd")])
            k.dma("act", g.gsd[0], g.gs1[:].rearrange("p l k s -> p (l k s)"), reads=["gs"], writes=[("dram", "gsd")])
            k.dma("sp", g.gsd[1], g.gs2[:].rearrange("p l k s -> p (l k s)"), reads=["gs"], writes=[("dram", "gsd")])
            if "modsd" in dump:
                k.dma("sp", g.modsd, g.mods[:].rearrange("p l j k s -> p (l j k s)"), reads=["mods"], writes=[("dram", "modsd")])
                outs.append(("dram", "modsd"))
        if on("lb"):
            phase_lb(g)
        drain(k)
        for li, l in enumerate(layers):
            xsrc = g.xT0 if (li == 0 and "xT" not in feed) else g.xT
            if on("norm1"):
                reload()
                src = xsrc
                phase_norm(g, src, g.hT, BF16, lambda kk, s: g.gs1[:, l, kk, s:s + 1], lambda kk, s: g.mods[:, l, 0, kk, s:s + 1], "n1")
                drain(k)
            if on("inproj"):
                phase_inproj(g, l, mchunks=getattr(make_program, "mchunks", None))
                drain(k)
            if on("hgrn"):
                phase_hgrn(g, l, heads=getattr(make_program, "heads", range(8)))
                drain(k)
            if on("conv"):
                phase_conv(g, l, chunks=getattr(make_program, "cchunks", range(8)))
                drain(k)
            if on("attn"):
                phase_attn(g, l, kvheads=getattr(make_program, "kvheads", range(2)), qsub=getattr(make_program, "qsub", range(4)))
                drain(k)
            if on("merge"):
                phase_merge(g, l)
                drain(k)
            if on("wout"):
                reload()
                phase_wout(g, l, xsrc, g.xT)
                drain(k)
            if getattr(make_program, "dumpx", False):
                xm = nc.dram_tensor("xm%d" % l, [D, T], F32, kind="ExternalOutput").ap()
                for q4 in range(4):
                    k.dma(None, xm[q4 * 512:(q4 + 1) * 512, :], g.xT[q4 * 512:(q4 + 1) * 512, :], reads=[("dram", "xT")], writes=[("dram", "xm")])
                drain(k)
            if on("norm2"):
                reload()
                phase_norm2_router(g, l)
                drain(k)
            if on("route"):
                phase_route(g, l)
                drain(k)
            if on("experts"):
                phase_experts(g, l, experts=getattr(make_program, "experts", range(NE)))
                drain(k)
            if on("combine"):
                reload()
                phase_combine(g, l)
                drain(k)
            if getattr(make_program, "dumpx", False):
                xd = nc.dram_tensor("xd%d" % l, [D, T], F32, kind="ExternalOutput").ap()
                for q4 in range(4):
                    k.dma(None, xd[q4 * 512:(q4 + 1) * 512, :], g.xT[q4 * 512:(q4 + 1) * 512, :], reads=[("dram", "xT")], writes=[("dram", "xd")])
                drain(k)
        if on("final"):
            phase_norm(g, g.xT, g.outT, F32, lambda kk, s: g.fng[:, kk:kk + 1], None, "nf", blocks=BLOCKS[1:], dst_off=TC)
        drain(k)
        for e in ("pe", "dve", "act", "pool"):
            k._wait("sp", (k.sem[e], k.cnt[e])) if k.cnt[e] > 0 else None
    k.es.close()
    return nc, k


def phase_lb(g):
    k, nc = g.k, g.nc
    with ExitStack() as st:
        e = k.sb(st, "lbe", [128, DEPTH, 16], F32)
        ssum = k.sb(st, "lbs", [128, 16], F32)
        k.dma("sp", e[:], g.lbl_d, writes=["lbe"])
        k.op("act", lambda: nc.scalar.activation(out=e[:], in_=e[:], func=AF.Exp), reads=["lbe"], writes=["lbe"])
        k.op("dve", lambda: nc.vector.tensor_tensor(out=ssum[:], in0=e[:, 0, :], in1=e[:, 1, :], op=ALU.add), reads=["lbe"], writes=["lbs"])
        for l in (2, 3):
            k.op("dve", lambda: nc.vector.tensor_tensor(out=ssum[:], in0=ssum[:], in1=e[:, l, :], op=ALU.add), reads=["lbe", "lbs"], writes=["lbs"])
        k.op("dve", lambda: nc.vector.reciprocal(out=ssum[:], in_=ssum[:]), reads=["lbs"], writes=["lbs"])
        lb = g.lb
        k.op("dve", lambda: nc.vector.memset(lb[:, 0, :], 0.0), writes=["lb"])
        for l in (1, 2, 3):
            k.op("dve", lambda: nc.vector.tensor_tensor(out=e[:, l, :], in0=e[:, l, :], in1=ssum[:], op=ALU.mult), reads=["lbe", "lbs"], writes=["lbe"])
            k.op("dve", lambda: nc.vector.tensor_tensor(out=lb[:, l, :], in0=lb[:, l - 1, :], in1=e[:, l, :], op=ALU.add), reads=["lbe", "lb"], writes=["lb"])
        k.op("dve", lambda: nc.vector.tensor_scalar(out=g.oml[:], in0=lb[:], scalar1=-1.0, scalar2=1.0, op0=ALU.mult, op1=ALU.add),
             reads=["lb"], writes=["oml"])


HSEGS = [(0, TC)] + [(TC + 2048 * i, 2048) for i in range(TL // 2048)]


def phase_hgrn(g, l, heads=range(8)):
    k, nc = g.k, g.nc
    CH = 64
    with ExitStack() as st:
        WM = 2048
        q = k.sb(st, "hq", [128, WM], BF16)
        v = k.sb(st, "hv", [128, WM], BF16)
        gsl = k.sb(st, "hg", [128, WM], BF16)
        f = k.sb(st, "hf", [128, WM], F32)
        kk = k.sb(st, "hkk", [128, WM], F32)
        lf = k.sb(st, "hlf", [128, WM], F32)
        b = k.sb(st, "hb", [128, WM], F32)
        bx = k.sb(st, "hbx", [128, WM], F32)
        dd = k.sb(st, "hd", [128, WM], F32)
        E = k.sb(st, "hE", [128, WM], F32)
        qd = k.sb(st, "hqd", [128, WM], BF16)
        kd = k.sb(st, "hkd", [128, WM], BF16)
        qin = k.sb(st, "hqin", [128, WM], BF16)
        kend = k.sb(st, "hkend", [128, WM], BF16)
        dec = k.sb(st, "hdec", [128, WM // CH], F32)
        rmask = k.sb(st, "hrm", [128, WM], F32)
        qd2 = k.sb(st, "hqd2", [128, WM], BF16)
        kd2 = k.sb(st, "hkd2", [128, WM], BF16)
        vtok = k.sb(st, "hvtok", [32, WM // CH, 2, 128], BF16)
        ktok = k.sb(st, "hktok", [32, WM // CH, 2, 128], BF16)
        am = k.sb(st, "ham", [32, WM // CH, 2, CH], BF16)
        osb = k.sb(st, "hosb", [128, WM], F32)
        ofw = k.sb(st, "hofw", [128, WM], F32)
        sq = k.sb(st, "hsq", [128, WM], BF16)
        rs = k.sb(st, "hrs", [128, 512], F32)
        yout = k.sb(st, "hy", [128, WM], BF16)
        S32 = k.sb(st, "hS32", [128, 128], F32)
        Sbf = [k.sb(st, "hSbf%d" % i, [128, 128], BF16) for i in range(2)]
        mAf = k.sb(st, "hmAf", [32, 64], BF16)
        mBb = k.sb(st, "hmBb", [32, 64], BF16)
        ptv = k.ps(st, "hptv", [32, 4, 2, 128], BF16)
        ptk = k.ps(st, "hptk", [32, 4, 2, 128], BF16)
        pat = k.ps(st, "hpat", [32, 4, 2, CH])
        pkv = [k.ps(st, "hpkv%d" % i, [128, 4, 128]) for i in range(2)]
        po = [k.ps(st, "hpo%d" % i, [128, 512]) for i in range(2)]
        k.op("dve", lambda: nc.vector.memset(rmask[:], 1.0), writes=["hrm"])
        k.op("dve", lambda: nc.vector.memset(rmask[:].rearrange("p (c j) -> p c j", j=CH)[:, :, 0:1], 0.0), writes=["hrm"])
        k.op("dve", lambda: nc.vector.memset(mAf[:], 1.0), writes=["hmask"])
        k.op("dve", lambda: nc.vector.memset(mBb[:], 1.0), writes=["hmask"])
        k.op("dve", lambda: nc.vector.tensor_copy(out=mAf[:, 0:32], in_=g.cst[0:32, 128:160]), reads=["cst"], writes=["hmask"])
        k.op("dve", lambda: nc.vector.tensor_copy(out=mBb[:, 32:64], in_=g.cst[0:32, 256:288]), reads=["cst"], writes=["hmask"])
        sbi = 0
        heads = list(heads)
        for h in [heads[0]] + heads:
            for di in (0, 1):
                segs = HSEGS if di == 0 else [HSEGS[0]] + HSEGS[:0:-1]
                zrow = (di * 8 + h) * 128
                lbi = di * 8 + h
                k.op("dve", lambda: nc.vector.memset(S32[:], 0.0), writes=["hS32"])
                k.op("pool", lambda: nc.gpsimd.memset(am[:], 0.0), writes=[("ham", i) for i in range(WM // CH // 4)])
                k.op("dve", lambda: nc.vector.memset(Sbf[sbi % 2][:], 0.0), writes=[("hSbf", sbi % 2)])
                for (c0, W) in segs:
                    NCH = W // CH
                    c3 = lambda t: t[:, :W].rearrange("p (c j) -> p c j", j=CH)
                    k.dma(None, q[:, :W], g.pT[h * 128:(h + 1) * 128, c0:c0 + W], reads=[("dram", "pT")], writes=["hq"])
                    k.dma(None, v[:, :W], g.pT[(8 + h) * 128:(9 + h) * 128, c0:c0 + W], reads=[("dram", "pT")], writes=["hv"])
                    k.dma(None, f[:, :W], g.zT[zrow:zrow + 128, c0:c0 + W], reads=[("dram", "zT")], writes=["hf"])
                    if di == 1:
                        k.dma(None, gsl[:, :W], g.pT[(32 + h) * 128:(33 + h) * 128, c0:c0 + W], reads=[("dram", "pT")], writes=["hg"])
                        k.dma(None, ofw[:, :W], g.oT[h * 128:(h + 1) * 128, c0:c0 + W], reads=[("dram", "oT")], writes=["hofw"])
                    k.op("dve", lambda: nc.vector.tensor_scalar(out=f[:, :W], in0=f[:, :W], scalar1=g.oml[:, l, lbi:lbi + 1], scalar2=g.lb[:, l, lbi:lbi + 1],
                                                                op0=ALU.mult, op1=ALU.add), reads=["hf", "lb", "oml"], writes=["hf"])
                    k.op("dve", lambda: nc.vector.tensor_scalar(out=kk[:, :W], in0=f[:, :W], scalar1=-1.0, scalar2=1.0, op0=ALU.mult, op1=ALU.add),
                         reads=["hf"], writes=["hkk"])
                    k.op("act", lambda: nc.scalar.activation(out=lf[:, :W], in_=f[:, :W], func=AF.Ln), reads=["hf"], writes=["hlf"])
                    k.op("dve", lambda: nc.vector.tensor_tensor_scan(out=b[:, :W], data0=rmask[:, :W], data1=lf[:, :W], initial=0.0, op0=ALU.mult, op1=ALU.add),
                         reads=["hlf", "hrm"], writes=["hb"])
                    if di == 0:
                        bxx = b
                        kbx = "hb"
                        mid, end = 31, 63
                    else:
                        k.op("dve", lambda: nc.vector.tensor_tensor(out=c3(dd), in0=c3(b), in1=c3(b)[:, :, 63:64].to_broadcast([128, NCH, CH]), op=ALU.subtract),
                             reads=["hb"], writes=["hd"])
                        k.op("dve", lambda: nc.vector.tensor_tensor(out=bx[:, :W], in0=lf[:, :W], in1=dd[:, :W], op=ALU.subtract),
                             reads=["hlf", "hd"], writes=["hbx"])
                        bxx = bx
                        kbx = "hbx"
                        mid, end = 32, 0
                    c4v = lambda t: t[:, :W].rearrange("p (c s j) -> p c s j", s=2, j=32)
                    k.op("dve", lambda: nc.vector.tensor_tensor(out=c4v(dd), in0=c4v(bxx), in1=c4v(bxx)[:, :, :, 15:16].to_broadcast([128, NCH, 2, 32]), op=ALU.subtract),
                         reads=[kbx], writes=["hd"])
                    k.op("act", lambda: nc.scalar.activation(out=E[:, :W], in_=dd[:, :W], func=AF.Exp), reads=["hd"], writes=["hE"])
                    k.op("dve", lambda: nc.vector.tensor_tensor(out=qd[:, :W], in0=q[:, :W], in1=E[:, :W], op=ALU.mult), reads=["hq", "hE"], writes=["hqd"])
                    k.op("act", lambda: nc.scalar.activation(out=E[:, :W], in_=dd[:, :W], func=AF.Exp, scale=-1.0), reads=["hd"], writes=["hE"])
                    k.op("dve", lambda: nc.vector.tensor_tensor(out=kd[:, :W], in0=kk[:, :W], in1=E[:, :W], op=ALU.mult), reads=["hkk", "hE"], writes=["hkd"])
                    k.op("dve", lambda: nc.vector.tensor_tensor(out=c3(dd), in0=c3(bxx), in1=c3(bxx)[:, :, mid:mid + 1].to_broadcast([128, NCH, CH]), op=ALU.subtract),
                         reads=[kbx], writes=["hd"])
                    k.op("act", lambda: nc.scalar.activation(out=E[:, :W], in_=dd[:, :W], func=AF.Exp), reads=["hd"], writes=["hE"])
                    k.op("dve", lambda: nc.vector.tensor_tensor(out=qd2[:, :W], in0=q[:, :W], in1=E[:, :W], op=ALU.mult), reads=["hq", "hE"], writes=["hqd2"])
                    k.op("act", lambda: nc.scalar.activation(out=E[:, :W], in_=dd[:, :W], func=AF.Exp, scale=-1.0), reads=["hd"], writes=["hE"])
                    k.op("dve", lambda: nc.vector.tensor_tensor(out=kd2[:, :W], in0=kk[:, :W], in1=E[:, :W], op=ALU.mult), reads=["hkk", "hE"], writes=["hkd2"])
                    k.op("act", lambda: nc.scalar.activation(out=E[:, :W], in_=bxx[:, :W], func=AF.Exp), reads=[kbx], writes=["hE"])
                    k.op("dve", lambda: nc.vector.tensor_tensor(out=qin[:, :W], in0=q[:, :W], in1=E[:, :W], op=ALU.mult), reads=["hq", "hE"], writes=["hqin"])
                    k.op("dve", lambda: nc.vector.tensor_tensor(out=c3(dd), in0=c3(bxx), in1=c3(bxx)[:, :, end:end + 1].to_broadcast([128, NCH, CH]), op=ALU.subtract),
                         reads=[kbx], writes=["hd"])
                    k.op("act", lambda: nc.scalar.activation(out=E[:, :W], in_=dd[:, :W], func=AF.Exp, scale=-1.0), reads=["hd"], writes=["hE"])
                    k.op("dve", lambda: nc.vector.tensor_tensor(out=kend[:, :W], in0=kk[:, :W], in1=E[:, :W], op=ALU.mult), reads=["hkk", "hE"], writes=["hkend"])
                    k.op("act", lambda: nc.scalar.activation(out=dec[:, :NCH], in_=c3(bxx)[:, :, end], func=AF.Exp), reads=[kbx], writes=["hdec"])
                    for c4 in range(0, NCH, 4):
                        for j in range(4):
                            for sbk in range(2):
                                cs = (c4 + j) * CH + sbk * 32
                                k.op("pe", lambda: nc.tensor.transpose(out=ptv[:, j, sbk, :], in_=v[:, cs:cs + 32], identity=g.ident_bf[:]), reads=["hv", "ident"], writes=["hptv"])
                                k.op("pe", lambda: nc.tensor.transpose(out=ptk[:, j, sbk, :], in_=kend[:, cs:cs + 32], identity=g.ident_bf[:]), reads=["hkend", "ident"], writes=["hptk"])
                        k.op("dve", lambda: nc.vector.tensor_copy(out=vtok[:, c4:c4 + 4, :, :], in_=ptv[:]), reads=["hptv"], writes=[("hvtok", c4 // 4)])
                        k.op("act", lambda: nc.scalar.copy(out=ktok[:, c4:c4 + 4, :, :], in_=ptk[:]), reads=["hptk"], writes=[("hktok", c4 // 4)])
                        for j in range(4):
                            ca = (c4 + j) * CH
                            cb_ = ca + 32
                            mm = lambda o, lh, rh: k.op("pe", lambda: nc.tensor.matmul(o, lhsT=lh, rhs=rh, start=True, stop=True),
                                                         reads=["hkd", "hqd", "hkd2", "hqd2"], writes=["hpat"])
                            mm(pat[:, j, 0, 0:32], kd[:, ca:ca + 32], qd[:, ca:ca + 32])
                            mm(pat[:, j, 1, 32:64], kd[:, cb_:cb_ + 32], qd[:, cb_:cb_ + 32])
                            if di == 0:
                                mm(pat[:, j, 0, 32:64], kd2[:, ca:ca + 32], qd2[:, cb_:cb_ + 32])
                            else:
                                mm(pat[:, j, 1, 0:32], kd2[:, cb_:cb_ + 32], qd2[:, ca:ca + 32])
                        if di == 0:
                            k.op("dve", lambda: nc.vector.tensor_tensor(out=am[:, c4:c4 + 4, 0, :], in0=pat[:, :, 0, :], in1=mAf[:].unsqueeze(1).to_broadcast([32, 4, 64]), op=ALU.mult),
                                 reads=["hpat", "hmask"], writes=[("ham", c4 // 4)])
                            k.op("dve", lambda: nc.vector.tensor_tensor(out=am[:, c4:c4 + 4, 1, 32:64], in0=pat[:, :, 1, 32:64], in1=mAf[:, 0:32].unsqueeze(1).to_broadcast([32, 4, 32]), op=ALU.mult),
                                 reads=["hpat", "hmask"], writes=[("ham", c4 // 4)])
                        else:
                            k.op("dve", lambda: nc.vector.tensor_tensor(out=am[:, c4:c4 + 4, 1, :], in0=pat[:, :, 1, :], in1=mBb[:].unsqueeze(1).to_broadcast([32, 4, 64]), op=ALU.mult),
                                 reads=["hpat", "hmask"], writes=[("ham", c4 // 4)])
                            k.op("dve", lambda: nc.vector.tensor_tensor(out=am[:, c4:c4 + 4, 0, 0:32], in0=pat[:, :, 0, 0:32], in1=mBb[:, 32:64].unsqueeze(1).to_broadcast([32, 4, 32]), op=ALU.mult),
                                 reads=["hpat", "hmask"], writes=[("ham", c4 // 4)])
                    order = list(range(NCH)) if di == 0 else list(range(NCH - 1, -1, -1))
                    for oi, c in enumerate(order):
                        cs = c * CH
                        pk = pkv[(oi // 4) % 2]
                        kpk = ("hpkv", (oi // 4) % 2)
                        for sbk in range(2):
                            k.op("pe", lambda: nc.tensor.matmul(pk[:, oi % 4, :], lhsT=ktok[:, c, sbk, :], rhs=vtok[:, c, sbk, :], start=(sbk == 0), stop=(sbk == 1)),
                                 reads=[("hktok", c // 4), ("hvtok", c // 4)], writes=[kpk])
                        pb = po[(c // 8) % 2]
                        kpo = ("hpo", (c // 8) % 2)
                        oc = (c % 8) * CH
                        for sbk in range(2):
                            k.op("pe", lambda: nc.tensor.matmul(pb[:, oc:oc + CH], lhsT=vtok[:, c, sbk, :], rhs=am[:, c, sbk, :], start=(sbk == 0), stop=False),
                                 reads=[("hvtok", c // 4), ("ham", c // 4)], writes=[kpo])
                        k.op("pe", lambda: nc.tensor.matmul(pb[:, oc:oc + CH], lhsT=Sbf[sbi % 2][:], rhs=qin[:, cs:cs + CH], start=False, stop=True),
                             reads=[("hSbf", sbi % 2), "hqin"], writes=[kpo])
                        k.op("dve", lambda: nc.vector.scalar_tensor_tensor(out=S32[:], in0=S32[:], scalar=dec[:, c:c + 1], in1=pk[:, oi % 4, :], op0=ALU.mult, op1=ALU.add),
                             reads=["hS32", "hdec", kpk], writes=["hS32"])
                        sbi += 1
                        k.op("pool", lambda: nc.gpsimd.tensor_copy(out=Sbf[sbi % 2][:], in_=S32[:]), reads=["hS32"], writes=[("hSbf", sbi % 2)])
                        last_in_bank = (c % 8 == 7) if di == 0 else (c % 8 == 0)
                        if last_in_bank or NCH < 8 and oi == NCH - 1:
                            b0 = (c // 8) * 512
                            bw = min(512, W - b0)
                            k.op("act", lambda: nc.scalar.copy(out=osb[:, b0:b0 + bw], in_=pb[:, :bw]), reads=[kpo], writes=["hosb"])
                    if getattr(g, "dbg", False) and di == 0 and h == 0:
                        for ri, (tt, kt) in enumerate(((f, "hf"), (lf, "hlf"), (b, "hb"), (kk, "hkk"), (E, "hE"), (osb, "hosb"))):
                            k.dma("sp", g.ybT[ri * 128:(ri + 1) * 128, c0:c0 + W], tt[:, :W], reads=[kt], writes=[("dram", "ybT")])
                        k.dma("sp", g.ybT[768:896, 0:16], g.lb[:, l, :], reads=["lb"], writes=[("dram", "ybT")])
                        k.dma("sp", g.ybT[896:1024, 0:128], S32[:], reads=["hS32"], writes=[("dram", "ybT")])
                    if di == 0:
                        k.dma(None, g.oT[h * 128:(h + 1) * 128, c0:c0 + W], osb[:, :W], reads=["hosb"], writes=[("dram", "oT")])
                    else:
                        k.op("dve", lambda: nc.vector.tensor_tensor(out=osb[:, :W], in0=osb[:, :W], in1=ofw[:, :W], op=ALU.add), reads=["hosb", "hofw"], writes=["hosb"])
                        k.op("act", lambda: nc.scalar.activation(out=sq[:, :W], in_=osb[:, :W], func=AF.Square), reads=["hosb"], writes=["hsq"])
                        for b0 in range(0, W, 512):
                            bw = min(512, W - b0)
                            pb = po[(b0 // 512) % 2]
                            kpo = ("hpo", (b0 // 512) % 2)
                            k.op("pe", lambda: nc.tensor.matmul(pb[:, :bw], lhsT=g.ones_bf[:], rhs=sq[:, b0:b0 + bw], start=True, stop=True), reads=["hsq", "ones"], writes=[kpo])
                            k.op("dve", lambda: nc.vector.tensor_scalar(out=rs[:, :bw], in0=pb[:, :bw], scalar1=1.0 / 128, scalar2=EPS, op0=ALU.mult, op1=ALU.add),
                                 reads=[kpo], writes=["hrs"])
                            k.op("dve", lambda: nc.vector.reciprocal(out=rs[:, :bw], in_=rs[:, :bw]), reads=["hrs"], writes=["hrs"])
                            k.op("act", lambda: nc.scalar.activation(out=rs[:, :bw], in_=rs[:, :bw], func=AF.Sqrt), reads=["hrs"], writes=["hrs"])
                            k.op("dve", lambda: nc.vector.tensor_tensor(out=osb[:, b0:b0 + bw], in0=osb[:, b0:b0 + bw], in1=rs[:, :bw], op=ALU.mult),
                                 reads=["hosb", "hrs"], writes=["hosb"])
                        k.op("dve", lambda: nc.vector.scalar_tensor_tensor(out=yout[:, :W], in0=osb[:, :W], scalar=g.ong[:, l, h:h + 1], in1=gsl[:, :W], op0=ALU.mult, op1=ALU.mult),
                             reads=["hosb", "hg", "ong"], writes=["hy"])
                        k.dma(None, g.yT[h * 128:(h + 1) * 128, c0:c0 + W], yout[:, :W], reads=["hy"], writes=[("dram", "yT")])
                if di == 0:
                    drain(k)


def phase_conv(g, l, chunks=range(8)):
    k, nc = g.k, g.nc
    PAD = 15
    with ExitStack() as st:
        a = k.sb(st, "cva", [128, T], BF16)
        sg = k.sb(st, "cvs", [128, T], BF16)
        hc = k.sb(st, "cvhc", [128, TC + 2 * PAD], BF16)
        hl = k.sb(st, "cvhl", [128, TL + 2 * PAD], BF16)
        dg = k.sb(st, "cvdg", [128, 31, 128], BF16)
        ot = [k.sb(st, "cvo%d" % i, [128, 512], F32) for i in range(2)]
        ps = [k.ps(st, "cvps%d" % i, [128, 512]) for i in range(2)]
        k.op("pool", lambda: nc.gpsimd.memset(hc[:], 0.0), writes=["cvhc"])
        k.op("pool", lambda: nc.gpsimd.memset(hl[:], 0.0), writes=["cvhl"])
        bi = 0
        for j in chunks:
            k.dma(None, a[:], g.pT[(40 + j) * 128:(41 + j) * 128, :], reads=[("dram", "pT")], writes=["cva"])
            k.dma(None, sg[:], g.pT[(48 + j) * 128:(49 + j) * 128, :], reads=[("dram", "pT")], writes=["cvs"])
            k.op("dve", lambda: nc.vector.tensor_tensor(out=hc[:, PAD:PAD + TC], in0=a[:, :TC], in1=sg[:, :TC], op=ALU.mult), reads=["cva", "cvs"], writes=["cvhc"])
            k.op("pool", lambda: nc.gpsimd.tensor_tensor(out=hl[:, PAD:PAD + TL], in0=a[:, TC:], in1=sg[:, TC:], op=ALU.mult), reads=["cva", "cvs"], writes=["cvhl"])
            for tap in range(31):
                k.op("dve", lambda: nc.vector.tensor_scalar(out=dg[:, tap, :], in0=g.ident_bf[:], scalar1=g.cw[:, l, j, tap:tap + 1], scalar2=None, op0=ALU.mult),
                     reads=["ident", "cw"], writes=["cvdg"])
            for (c0, w) in BLOCKS:
                src, t0, ks = (hc, c0, "cvhc") if c0 < TC else (hl, c0 - TC, "cvhl")
                p = ps[bi % 2]
                o = ot[bi % 2]
                for tap in range(31):
                    k.op("pe", lambda: nc.tensor.matmul(p[:, :w], lhsT=dg[:, tap, :], rhs=src[:, t0 + tap:t0 + tap + w], start=(tap == 0), stop=(tap == 30)),
                         reads=["cvdg", ks], writes=[("cvps", bi % 2)])
                k.op("act", lambda: nc.scalar.activation(out=o[:, :w], in_=p[:, :w], func=AF.Identity, bias=g.cb[:, l, j:j + 1]),
                     reads=[("cvps", bi % 2), "cw"], writes=[("cvo", bi % 2)])
                k.dma(None, g.ybT[j * 128:(j + 1) * 128, c0:c0 + w], o[:, :w], reads=[("cvo", bi % 2)], writes=[("dram", "ybT")])
                bi += 1


def phase_attn(g, l, kvheads=range(2), qsub=range(4)):
    k, nc = g.k, g.nc
    NLT = TL // 128
    scale = 128 ** -0.5
    with ExitStack() as st:
        cosT = k.sb(st, "atcos", [128, TL], F32)
        sinT = k.sb(st, "atsin", [128, TL], F32)
        rm = k.sb(st, "atrm", [128, 128], BF16)
        mge = k.sb(st, "atmge", [128, 128], BF16)
        mle = k.sb(st, "atmle", [128, 128], BF16)
        kr = k.sb(st, "atk", [128, T], BF16)
        vT = k.sb(st, "atv", [128, T], BF16)
        qr = k.sb(st, "atq", [128, T], BF16)
        vtok = k.sb(st, "atvtok", [128, T // 128, 128], BF16)
        t1 = k.sb(st, "att1", [128, 512], F32)
        t2 = k.sb(st, "att2", [128, 512], F32)
        pts = [k.sb(st, "atp%d" % i, [128, 5, 128], BF16) for i in range(2)]
        rden = k.sb(st, "atrden", [128, 128], F32)
        yc = k.sb(st, "atyc", [128, T], BF16)
        esk = k.sb(st, "atesk", [128, 8], F32)
        prot = k.ps(st, "atprot", [128, 512])
        ptr = k.ps(st, "atptr", [128, 4, 128], BF16)
        psl = [k.ps(st, "atpsl%d" % i, [128, 3, 128]) for i in range(2)]
        psc = [k.ps(st, "atpsc%d" % i, [128, 2, 128]) for i in range(2)]
        pso = [k.ps(st, "atpso%d" % i, [128, 2, 128]) for i in range(2)]
        k.dma("sp", cosT[:], g.rope_d[0], writes=["atcos"])
        k.dma("act", sinT[:], g.rope_d[1], writes=["atsin"])
        k.op("dve", lambda: nc.vector.tensor_copy(out=rm[:], in_=g.cst[:, 384:512]), reads=["cst"], writes=["atrm"])
        k.op("dve", lambda: nc.vector.tensor_copy(out=mle[:], in_=g.cst[:, 128:256]), reads=["cst"], writes=["atm"])
        k.op("dve", lambda: nc.vector.tensor_copy(out=mge[:], in_=g.cst[:, 256:384]), reads=["cst"], writes=["atm"])
        k.op("act", lambda: nc.scalar.activation(out=esk[:], in_=g.sink[:, l, :], func=AF.Exp), reads=["sink"], writes=["atesk"])

        def rope(dst, src, ksrc, kdst):
            for b0 in range(0, TL, 512):
                xs = src[:, TC + b0:TC + b0 + 512]
                k.op("pe", lambda: nc.tensor.matmul(prot[:], lhsT=rm[:], rhs=xs, start=True, stop=True), reads=[ksrc, "atrm"], writes=["atprot"])
                k.op("dve", lambda: nc.vector.tensor_tensor(out=t1[:], in0=prot[:], in1=sinT[:, b0:b0 + 512], op=ALU.mult), reads=["atprot", "atsin"], writes=["att1"])
                k.op("pool", lambda: nc.gpsimd.tensor_tensor(out=t2[:], in0=xs, in1=cosT[:, b0:b0 + 512], op=ALU.mult), reads=[ksrc, "atcos"], writes=["att2"])
                k.op("dve", lambda: nc.vector.tensor_tensor(out=dst[:, TC + b0:TC + b0 + 512], in0=t1[:], in1=t2[:], op=ALU.add), reads=["att1", "att2"], writes=[kdst])

        bi = 0
        for kvh in kvheads:
            k.dma(None, kr[:], g.pT[(64 + kvh) * 128:(65 + kvh) * 128, :], reads=[("dram", "pT")], writes=["atk"])
            k.dma(None, vT[:], g.pT[(66 + kvh) * 128:(67 + kvh) * 128, :], reads=[("dram", "pT")], writes=["atv"])
            rope(kr, kr, "atk", "atk")
            for t4 in range(0, T // 128, 4):
                n4 = min(4, T // 128 - t4)
                for j in range(n4):
                    cs = (t4 + j) * 128
                    k.op("pe", lambda: nc.tensor.transpose(out=ptr[:, j, :], in_=vT[:, cs:cs + 128], identity=g.ident_bf[:]), reads=["atv", "ident"], writes=["atptr"])
                k.op("dve", lambda: nc.vector.tensor_copy(out=vtok[:, t4:t4 + n4, :], in_=ptr[:, :n4, :]), reads=["atptr"], writes=["atvtok"])
            for qs in qsub:
                h = kvh * 4 + qs
                k.dma(None, qr[:], g.pT[(56 + h) * 128:(57 + h) * 128, :], reads=[("dram", "pT")], writes=["atq"])
                rope(qr, qr, "atq", "atq")
                qblocks = [("lat", n) for n in range(NLT)] + [("ctx", n) for n in range(TC // 128)]
                for (kind, n) in qblocks:
                    pt = pts[bi % 2]
                    kpt = ("atp", bi % 2)
                    sl, sc, so = psl[bi % 2], psc[bi % 2], pso[bi % 2]
                    ksl, ksc, kso = ("atpsl", bi % 2), ("atpsc", bi % 2), ("atpso", bi % 2)
                    bi += 1
                    if kind == "lat":
                        qc = TC + 128 * n
                        loc = [(s, n - 1 + s) for s in range(3) if 0 <= n - 1 + s < NLT]
                    else:
                        qc = 128 * n
                        loc = []
                    qv = qr[:, qc:qc + 128]
                    for (s, tn) in loc:
                        kc = TC + 128 * tn
                        k.op("pe", lambda: nc.tensor.matmul(sl[:, s, :], lhsT=kr[:, kc:kc + 128], rhs=qv, start=True, stop=True), reads=["atk", "atq"], writes=[ksl])
                    for s in range(2):
                        k.op("pe", lambda: nc.tensor.matmul(sc[:, s, :], lhsT=kr[:, 128 * s:128 * s + 128], rhs=qv, start=True, stop=True), reads=["atk", "atq"], writes=[ksc])
                    if loc:
                        s0, s1 = loc[0][0], loc[-1][0] + 1
                        k.op("act", lambda: nc.scalar.activation(out=pt[:, s0:s1, :], in_=sl[:, s0:s1, :], func=AF.Exp, scale=scale), reads=[ksl], writes=[kpt])
                        if s0 == 0:
                            k.op("pool", lambda: nc.gpsimd.tensor_tensor(out=pt[:, 0, :], in0=pt[:, 0, :], in1=mge[:], op=ALU.mult), reads=[kpt, "atm"], writes=[kpt])
                        if s1 == 3:
                            k.op("pool", lambda: nc.gpsimd.tensor_tensor(out=pt[:, 2, :], in0=pt[:, 2, :], in1=mle[:], op=ALU.mult), reads=[kpt, "atm"], writes=[kpt])
                    k.op("act", lambda: nc.scalar.activation(out=pt[:, 3:5, :], in_=sc[:], func=AF.Exp, scale=scale), reads=[ksc], writes=[kpt])
                    tiles = [(s, (TC // 128) + tn) for (s, tn) in loc] + [(3, 0), (4, 1)]
                    for i, (s, vt) in enumerate(tiles):
                        k.op("pe", lambda: nc.tensor.matmul(so[:, 0, :], lhsT=vtok[:, vt, :], rhs=pt[:, s, :], start=(i == 0), stop=(i == len(tiles) - 1)),
                             reads=["atvtok", kpt], writes=[kso])
                    for i, (s, vt) in enumerate(tiles):
                        k.op("pe", lambda: nc.tensor.matmul(so[:, 1, :], lhsT=g.ones_bf[:], rhs=pt[:, s, :], start=(i == 0), stop=(i == len(tiles) - 1)),
                             reads=["ones", kpt], writes=[kso])
                    k.op("dve", lambda: nc.vector.tensor_scalar(out=rden[:], in0=so[:, 1, :], scalar1=esk[:, h:h + 1], scalar2=None, op0=ALU.add), reads=[kso, "atesk"], writes=["atrden"])
                    k.op("dve", lambda: nc.vector.reciprocal(out=rden[:], in_=rden[:]), reads=["atrden"], writes=["atrden"])
                    k.op("dve", lambda: nc.vector.tensor_tensor(out=yc[:, qc:qc + 128], in0=so[:, 0, :], in1=rden[:], op=ALU.mult), reads=[kso, "atrden"], writes=["atyc"])
                k.dma(None, g.yT[(16 + h) * 128:(17 + h) * 128, :], yc[:], reads=["atyc"], writes=[("dram", "yT")])


def _pk(a):
    sh = a.shape
    return np.ascontiguousarray(np.moveaxis(a.reshape(sh[:-1] + (sh[-1] // 128, 128)), -1, 0))


def const_tables():
    cst = np.zeros((128, 512), np.float32)
    p = np.arange(128)[:, None]
    j = np.arange(128)[None, :]
    cst[:, 0:128] = (p == j)
    cst[:, 128:256] = (p <= j)
    cst[:, 256:384] = (p >= j)
    rm = np.zeros((128, 128), np.float32)
    for do in range(128):
        i = do % 64
        if i < 32:
            rm[do + 32, do] = -1.0
        else:
            rm[do - 32, do] = 1.0
    cst[:, 384:512] = rm
    pos = np.arange(TL)
    rows = (pos // 64).astype(np.float32)
    cols = (pos % 64).astype(np.float32)
    inv = (10000.0 ** (-(np.arange(0, 64, 2, dtype=np.float32)) / 64.0)).astype(np.float32)
    rope = np.zeros((2, 128, TL), np.float32)
    for d in range(128):
        base = rows if d < 64 else cols
        ang = (base * inv[(d % 64) % 32]).astype(np.float32)
        rope[0, d] = np.cos(ang)
        rope[1, d] = np.sin(ang)
    return cst, rope


def host_inputs(inp):
    m = {}
    x = np.asarray(inp["x"], np.float32)[0]
    ctx = np.asarray(inp["ctx"], np.float32)[0]
    m["xT0"] = np.ascontiguousarray(np.concatenate([ctx, x], 0).T)
    cc = np.stack([np.asarray(inp["c"], np.float32)[0], np.asarray(inp["c_ctx"], np.float32)], -1)
    m["cvec"] = np.ascontiguousarray(cc.reshape(KD, 128, 2).transpose(1, 0, 2))
    m["b_ada"] = np.ascontiguousarray(np.asarray(inp["b_ada"], np.float32).reshape(DEPTH, 6, KD, 128).transpose(3, 0, 1, 2))
    m["w_ada"] = np.asarray(inp["w_ada"], np.float32)
    m["n1g"] = np.ascontiguousarray(np.asarray(inp["norm1_g"], np.float32).reshape(DEPTH, KD, 128).transpose(2, 0, 1))
    m["n2g"] = np.ascontiguousarray(np.asarray(inp["norm2_g"], np.float32).reshape(DEPTH, KD, 128).transpose(2, 0, 1))
    m["w_in"] = np.asarray(inp["w_in"], np.float32)
    cst, rope = const_tables()
    m["cst"] = cst
    m["rope"] = rope
    m["lbl"] = np.ascontiguousarray(np.asarray(inp["hgrn_lb_logits"], np.float32).reshape(DEPTH, 2, 8, 128).transpose(3, 0, 1, 2).reshape(128, DEPTH, 16))
    m["ong"] = np.ascontiguousarray(np.asarray(inp["hgrn_onorm_g"], np.float32).reshape(DEPTH, 8, 128).transpose(2, 0, 1))
    m["cw"] = np.ascontiguousarray(np.asarray(inp["conv_w"], np.float32).reshape(DEPTH, 31, 8, 128).transpose(3, 0, 2, 1))
    m["cb"] = np.ascontiguousarray(np.asarray(inp["conv_b"], np.float32).reshape(DEPTH, 8, 128).transpose(2, 0, 1))
    m["lng"] = np.ascontiguousarray(np.asarray(inp["conv_ln_g"], np.float32).reshape(DEPTH, 8, 128).transpose(2, 0, 1))
    m["lnb"] = np.ascontiguousarray(np.asarray(inp["conv_ln_b"], np.float32).reshape(DEPTH, 8, 128).transpose(2, 0, 1))
    m["w_a"] = np.asarray(inp["w_branch_a"], np.float32)
    m["w_b"] = np.asarray(inp["w_branch_b"], np.float32)
    m["w_c"] = np.asarray(inp["w_branch_c"], np.float32)
    m["w_out"] = np.asarray(inp["w_out"], np.float32)
    m["w_router"] = np.asarray(inp["w_router"], np.float32)
    m["ebase"] = np.ascontiguousarray(np.broadcast_to((np.arange(NE, dtype=np.float32) * SROWS + SLOTS)[None], (128, NE)))
    m["fng"] = np.ascontiguousarray(np.asarray(inp["final_norm_g"], np.float32).reshape(KD, 128).T)
    m["w_eg"] = np.asarray(inp["w_exp_gate"], np.float32)
    m["w_eu"] = np.asarray(inp["w_exp_up"], np.float32)
    m["w_ed"] = np.asarray(inp["w_exp_down"], np.float32)
    m["sink"] = np.ascontiguousarray(np.broadcast_to(np.asarray(inp["attn_sink"], np.float32)[None], (128, DEPTH, 8)))
    return m


def phase_merge(g, l):
    k, nc = g.k, g.nc
    with ExitStack() as st:
        wb = [k.sb(st, "mgw%d" % i, [128, 8, D], BF16) for i in range(3)]
        stg = [k.sb(st, "mgstg%d" % i, [128, D], F32) for i in range(2)]
        ya = [k.sb(st, "mgya%d" % i, [128, 8, 512], BF16) for i in range(1)]
        yc = [k.sb(st, "mgyc%d" % i, [128, 8, 512], BF16) for i in range(1)]
        yb32 = k.sb(st, "mgyb32", [128, 8, 512], F32)
        ybs = k.sb(st, "mgybs", [128, 8, 512], BF16)
        yb = k.sb(st, "mgyb", [128, 8, 512], BF16)
        mean = k.sb(st, "mgmean", [128, 512], F32)
        var = k.sb(st, "mgvar", [128, 512], F32)
        gt = [k.sb(st, "mggt%d" % i, [128, 3, 512], BF16) for i in range(2)]
        acc = k.sb(st, "mgacc", [128, 512], F32)
        tmp = k.sb(st, "mgtmp", [128, 512], F32)
        mo = [k.sb(st, "mgmo%d" % i, [128, 512], BF16) for i in range(2)]
        pstat = [k.ps(st, "mgpst%d" % i, [128, 512]) for i in range(2)]
        pabc = [k.ps(st, "mgpabc%d" % i, [128, 512]) for i in range(6)]
        ci = 0
        for bi_, wsrc in enumerate((g.w_a, g.w_b, g.w_c)):
            for kk in range(8):
                sg = stg[ci % 2]
                k.dma(None, sg[:], wsrc[l][kk * 128:(kk + 1) * 128, :], writes=[("mgstg", ci % 2)])
                k.op("pool", lambda: nc.gpsimd.tensor_copy(out=wb[bi_][:, kk, :], in_=sg[:]), reads=[("mgstg", ci % 2)], writes=["mgw"])
                ci += 1
        it = 0
        for bi, (c0, w) in enumerate(BLOCKS):
            a_, c_ = ya[0], yc[0]
            ka, kc = ("mgya", 0), ("mgyc", 0)
            k.dma(None, a_[:, :, :w], g.yT[0:1024, :].rearrange("(k p) t -> p k t", p=128)[:, :, c0:c0 + w], reads=[("dram", "yT")], writes=[ka])
            k.dma(None, c_[:, :, :w], g.yT[2048:3072, :].rearrange("(k p) t -> p k t", p=128)[:, :, c0:c0 + w], reads=[("dram", "yT")], writes=[kc])
            k.dma(None, yb32[:, :, :w], fm(g.ybT, c0, w), reads=[("dram", "ybT")], writes=["mgyb32"])
            k.op("act", lambda: nc.scalar.copy(out=ybs[:, :, :w], in_=yb32[:, :, :w]), reads=["mgyb32"], writes=["mgybs"])
            for kk in range(8):
                k.op("pe", lambda: nc.tensor.matmul(pstat[0][:, :w], lhsT=g.ones_bf[:], rhs=ybs[:, kk, :w], start=(kk == 0), stop=(kk == 7)), reads=["mgybs", "ones"], writes=["mgpst0"])
            k.op("dve", lambda: nc.vector.tensor_scalar(out=mean[:, :w], in0=pstat[0][:, :w], scalar1=1.0 / 1024, scalar2=None, op0=ALU.mult), reads=["mgpst0"], writes=["mgmean"])
            k.op("dve", lambda: nc.vector.tensor_tensor(out=yb32[:, :, :w], in0=yb32[:, :, :w], in1=mean[:, :w].unsqueeze(1).to_broadcast([128, 8, w]), op=ALU.subtract),
                 reads=["mgyb32", "mgmean"], writes=["mgyb32"])
            k.op("act", lambda: nc.scalar.activation(out=ybs[:, :, :w], in_=yb32[:, :, :w], func=AF.Square), reads=["mgyb32"], writes=["mgybs"])
            for kk in range(8):
                k.op("pe", lambda: nc.tensor.matmul(pstat[1][:, :w], lhsT=g.ones_bf[:], rhs=ybs[:, kk, :w], start=(kk == 0), stop=(kk == 7)), reads=["mgybs", "ones"], writes=["mgpst1"])
            k.op("dve", lambda: nc.vector.tensor_scalar(out=var[:, :w], in0=pstat[1][:, :w], scalar1=1.0 / 1024, scalar2=EPS, op0=ALU.mult, op1=ALU.add), reads=["mgpst1"], writes=["mgvar"])
            k.op("dve", lambda: nc.vector.reciprocal(out=var[:, :w], in_=var[:, :w]), reads=["mgvar"], writes=["mgvar"])
            k.op("act", lambda: nc.scalar.activation(out=var[:, :w], in_=var[:, :w], func=AF.Sqrt), reads=["mgvar"], writes=["mgvar"])
            k.op("dve", lambda: nc.vector.tensor_tensor(out=yb32[:, :, :w], in0=yb32[:, :, :w], in1=var[:, :w].unsqueeze(1).to_broadcast([128, 8, w]), op=ALU.mult),
                 reads=["mgyb32", "mgvar"], writes=["mgyb32"])
            for kk in range(8):
                k.op("act", lambda: nc.scalar.activation(out=yb32[:, kk, :w], in_=yb32[:, kk, :w], func=AF.Identity, scale=g.lng[:, l, kk:kk + 1], bias=g.lnb[:, l, kk:kk + 1]),
                     reads=["mgyb32", "cw"], writes=["mgyb32"])
            k.op("act", lambda: nc.scalar.activation(out=yb[:, :, :w], in_=yb32[:, :, :w], func=AF.Silu), reads=["mgyb32"], writes=["mgyb"])
            for dc in range(KD):
                gg = gt[it % 2]
                kg = ("mggt", it % 2)
                o = mo[it % 2]
                ko = ("mgmo", it % 2)
                pa, pb, pc = pabc[(it % 2) * 3:(it % 2) * 3 + 3]
                kp = ("mgpabc", it % 2)
                it += 1
                for gi in range(3):
                    r0 = (68 + 16 * gi + dc) * 128
                    k.dma(None, gg[:, gi, :w], g.pT[r0:r0 + 128, c0:c0 + w], reads=[("dram", "pT")], writes=[kg])
                for (pp, src, ks, wi) in ((pa, a_, ka, 0), (pb, yb, "mgyb", 1), (pc, c_, kc, 2)):
                    for kk in range(8):
                        k.op("pe", lambda: nc.tensor.matmul(pp[:, :w], lhsT=wb[wi][:, kk, dc * 128:(dc + 1) * 128], rhs=src[:, kk, :w], start=(kk == 0), stop=(kk == 7)),
                             reads=[ks, "mgw"], writes=[kp])
                k.op("dve", lambda: nc.vector.tensor_tensor(out=acc[:, :w], in0=pa[:, :w], in1=gg[:, 0, :w], op=ALU.mult), reads=[kp, kg], writes=["mgacc"])
                k.op("dve", lambda: nc.vector.tensor_tensor(out=tmp[:, :w], in0=pb[:, :w], in1=gg[:, 1, :w], op=ALU.mult), reads=[kp, kg], writes=["mgtmp"])
                k.op("pool", lambda: nc.gpsimd.tensor_tensor(out=acc[:, :w], in0=acc[:, :w], in1=tmp[:, :w], op=ALU.add), reads=["mgacc", "mgtmp"], writes=["mgacc"])
                k.op("dve", lambda: nc.vector.tensor_tensor(out=tmp[:, :w], in0=pc[:, :w], in1=gg[:, 2, :w], op=ALU.mult), reads=[kp, kg], writes=["mgtmp"])
                k.op("pool", lambda: nc.gpsimd.tensor_tensor(out=o[:, :w], in0=acc[:, :w], in1=tmp[:, :w], op=ALU.add), reads=["mgacc", "mgtmp"], writes=[ko])
                k.dma(None, g.mT[dc * 128:(dc + 1) * 128, c0:c0 + w], o[:, :w], reads=[ko], writes=[("dram", "mT")])


def phase_wout(g, l, src_x, dst_x):
    k, nc = g.k, g.nc
    with ExitStack() as st:
        wb = k.sb(st, "wow", [128, KD, D], BF16)
        stg = [k.sb(st, "wostg%d" % i, [128, D], F32) for i in range(2)]
        mb = [k.sb(st, "wom%d" % i, [128, KD, 512], BF16) for i in range(2)]
        xb = [k.sb(st, "wox%d" % i, [128, KD, 512], F32) for i in range(2)]
        ps = [k.ps(st, "wops%d" % i, [128, 512]) for i in range(4)]
        for kk in range(KD):
            sg = stg[kk % 2]
            k.dma(None, sg[:], g.w_out[l][kk * 128:(kk + 1) * 128, :], writes=[("wostg", kk % 2)])
            k.op("pool", lambda: nc.gpsimd.tensor_copy(out=wb[:, kk, :], in_=sg[:]), reads=[("wostg", kk % 2)], writes=["wow"])
        pi = 0
        for bi, (c0, w) in enumerate(BLOCKS):
            s = 1 if c0 < TC else 0
            m_, x_ = mb[bi % 2], xb[bi % 2]
            km, kx = ("wom", bi % 2), ("wox", bi % 2)
            k.dma(None, m_[:, :, :w], fm(g.mT, c0, w), reads=[("dram", "mT")], writes=[km])
            k.dma(None, x_[:, :, :w], fm(src_x, c0, w), reads=[("dram", src_x.tensor.name)], writes=[kx])
            for dc in range(KD):
                p = ps[pi % 4]
                kp = ("wops", pi % 4)
                pi += 1
                for kk in range(KD):
                    k.op("pe", lambda: nc.tensor.matmul(p[:, :w], lhsT=wb[:, kk, dc * 128:(dc + 1) * 128], rhs=m_[:, kk, :w], start=(kk == 0), stop=(kk == KD - 1)),
                         reads=[km, "wow"], writes=[kp])
                k.op("dve", lambda: nc.vector.scalar_tensor_tensor(out=x_[:, dc, :w], in0=p[:, :w], scalar=g.mods[:, l, 2, dc, s:s + 1], in1=x_[:, dc, :w], op0=ALU.mult, op1=ALU.add),
                     reads=[kp, kx, "mods"], writes=[kx])
            k.dma(None, fm(dst_x, c0, w), x_[:, :, :w], reads=[kx], writes=[("dram", dst_x.tensor.name)])


def phase_norm2_router(g, l):
    k, nc = g.k, g.nc
    R = Ctx()

    def alloc(st):
        R.wr = k.sb(st, "rtw", [128, KD, NE], F32)
        R.ex = k.sb(st, "rtex", [NE, 512], F32)
        R.ri = k.sb(st, "rtri", [NE, 512], F32)
        R.af = k.sb(st, "rtaf", [NE, 512], F32)
        R.ones16 = k.sb(st, "rtones", [NE, NE], F32)
        R.hbf = k.sb(st, "rthbf", [128, KD, 512], BF16)
        R.htok = [k.sb(st, "rthtok%d" % i, [128, D], BF16) for i in range(2)]
        R.psr = k.ps(st, "rtpsr", [NE, 512])
        R.pss = k.ps(st, "rtpss", [NE, 512])
        R.ptt = k.ps(st, "rtptt", [128, KD, 128], BF16)
        R.ti = 0
        k.dma("sp", R.wr[:], g.w_router[l].rearrange("(k p) e -> p k e", p=128), writes=["rtw"])
        k.op("dve", lambda: nc.vector.memset(R.ones16[:], 1.0), writes=["rtones"])

    def extra(bi, c0, w, ho, kh):
        for kk in range(KD):
            k.op("pe", lambda: nc.tensor.matmul(R.psr[:, :w], lhsT=R.wr[:, kk, :], rhs=ho[:, kk, :w], start=(kk == 0), stop=(kk == KD - 1)),
                 reads=[kh, "rtw"], writes=["rtpsr"])
        k.op("act", lambda: nc.scalar.activation(out=R.ex[:, :w], in_=R.psr[:, :w], func=AF.Exp), reads=["rtpsr"], writes=["rtex"])
        k.op("pe", lambda: nc.tensor.matmul(R.pss[:, :w], lhsT=R.ones16[:], rhs=R.ex[:, :w], start=True, stop=True), reads=["rtex", "rtones"], writes=["rtpss"])
        k.op("dve", lambda: nc.vector.reciprocal(out=R.ri[:, :w], in_=R.pss[:, :w]), reads=["rtpss"], writes=["rtri"])
        k.op("dve", lambda: nc.vector.tensor_tensor(out=R.af[:, :w], in0=R.ex[:, :w], in1=R.ri[:, :w], op=ALU.mult), reads=["rtex", "rtri"], writes=["rtaf"])
        k.dma(None, g.affd[:, c0:c0 + w], R.af[:, :w], reads=["rtaf"], writes=[("dram", "affd")])
        k.op("pool", lambda: nc.gpsimd.tensor_copy(out=R.hbf[:, :, :w], in_=ho[:, :, :w]), reads=[kh], writes=["rthbf"])
        for tt in range(w // 128):
            for kk in range(KD):
                k.op("pe", lambda: nc.tensor.transpose(out=R.ptt[:, kk, :], in_=R.hbf[:, kk, tt * 128:(tt + 1) * 128], identity=g.ident_bf[:]),
                     reads=["rthbf", "ident"], writes=["rtptt"])
            ht = R.htok[R.ti % 2]
            kt = ("rthtok", R.ti % 2)
            R.ti += 1
            k.op("dve", lambda: nc.vector.tensor_copy(out=ht[:], in_=R.ptt[:].rearrange("p k f -> p (k f)")), reads=["rtptt"], writes=[kt])
            r0 = c0 + tt * 128
            k.dma(None, g.h2tok[r0:r0 + 128, :], ht[:], reads=[kt], writes=[("dram", "h2tok")])

    phase_norm(g, g.xT, None, F32, lambda kk, s: g.gs2[:, l, kk, s:s + 1], lambda kk, s: g.mods[:, l, 3, kk, s:s + 1], "n2",
               extra=extra, extra_alloc=alloc)


NBIS = 30
BIGI = float(1 << 22)


def phase_route(g, l):
    k, nc = g.k, g.nc
    with ExitStack() as st:
        al = k.sb(st, "ral", [128, NE, 64], F32)
        ac = k.sb(st, "rac", [128, NE, 2], F32)
        cml = k.sb(st, "rcml", [128, NE, 64], F32)
        cmc = k.sb(st, "rcmc", [128, NE, 2], F32)
        lo = k.sb(st, "rlo", [128, 32], F32)
        tcand = k.sb(st, "rtc", [128, 32], F32)
        cnt = k.sb(st, "rcnt", [128, 32], BF16)
        ge = k.sb(st, "rge", [128, 32], F32)
        kc = k.sb(st, "rkc", [128, 32], F32)
        rml = k.sb(st, "rrml", [128, NE, 64], F32)
        rmc = k.sb(st, "rrmc", [128, NE, 2], F32)
        csl = k.sb(st, "rcsl", [128, NE, 64], F32)
        csc = k.sb(st, "rcsc", [128, NE, 2], F32)
        tot = k.sb(st, "rtot", [128, 32], BF16)
        off = k.sb(st, "roff", [128, 32], F32)
        lstr = k.sb(st, "rlstr", [128, 128], BF16)
        gl = k.sb(st, "rgl", [128, NE, 64], F32)
        gc = k.sb(st, "rgc", [128, NE, 2], F32)
        sl = k.sb(st, "rsl", [128, NE, 64], F32)
        sc = k.sb(st, "rsc", [128, NE, 2], F32)
        gli = k.sb(st, "rgli", [128, NE, 64], I32)
        gci = k.sb(st, "rgci", [128, NE, 2], I32)
        sli = k.sb(st, "rsli", [128, NE, 64], I32)
        sci = k.sb(st, "rsci", [128, NE, 2], I32)
        ht = [k.sb(st, "rht%d" % i, [128, D], BF16) for i in range(3)]
        ptot = k.ps(st, "rptot", [128, 32])
        poff = k.ps(st, "rpoff", [128, 32])
        k.dma("sp", al[:], g.affd[:, TC:].rearrange("e (p f) -> p e f", f=64), reads=[("dram", "affd")], writes=["ral"])
        k.dma("act", ac[:], g.affd[:, 0:TC].rearrange("e (p f) -> p e f", f=2), reads=[("dram", "affd")], writes=["rac"])
        k.op("dve", lambda: nc.vector.memset(lo[:], 0.0), writes=["rlo"])
        k.op("dve", lambda: nc.vector.memset(kc[:, 0:16], float(CAPL)), writes=["rkc"])
        k.op("dve", lambda: nc.vector.memset(kc[:, 16:32], float(CAPC)), writes=["rkc"])
        k.op("dve", lambda: nc.vector.memset(rml[:], 1.0), writes=["rrm"])
        k.op("dve", lambda: nc.vector.memset(rml[:, :, 0:1], 0.0), writes=["rrm"])
        k.op("dve", lambda: nc.vector.memset(rmc[:], 1.0), writes=["rrm"])
        k.op("dve", lambda: nc.vector.memset(rmc[:, :, 0:1], 0.0), writes=["rrm"])
        k.op("dve", lambda: nc.vector.tensor_tensor(out=lstr[:], in0=g.cst[:, 128:256], in1=g.cst[:, 0:128], op=ALU.subtract), reads=["cst"], writes=["rlstr"])

        def masks(thr, kthr):
            k.op("dve", lambda: nc.vector.tensor_tensor(out=cml[:], in0=al[:], in1=thr[:, 0:16].unsqueeze(2).to_broadcast([128, NE, 64]), op=ALU.is_ge),
                 reads=["ral", kthr], writes=["rcml"])
            k.op("dve", lambda: nc.vector.tensor_tensor(out=cmc[:], in0=ac[:], in1=thr[:, 16:32].unsqueeze(2).to_broadcast([128, NE, 2]), op=ALU.is_ge),
                 reads=["rac", kthr], writes=["rcmc"])

        lowp = st.enter_context(nc.allow_low_precision("per-partition counts <= 64 are exact in bf16"))
        for it in range(NBIS):
            step = 2.0 ** (-(it + 1))
            k.op("dve", lambda: nc.vector.tensor_scalar(out=tcand[:], in0=lo[:], scalar1=step, scalar2=None, op0=ALU.add), reads=["rlo"], writes=["rtc"])
            masks(tcand, "rtc")
            k.op("dve", lambda: nc.vector.tensor_reduce(out=cnt[:, 0:16], in_=cml[:], axis=AX.X, op=ALU.add), reads=["rcml"], writes=["rcnt"])
            k.op("dve", lambda: nc.vector.tensor_reduce(out=cnt[:, 16:32], in_=cmc[:], axis=AX.X, op=ALU.add), reads=["rcmc"], writes=["rcnt"])
            k.op("pe", lambda: nc.tensor.matmul(ptot[:], lhsT=g.ones_bf[:], rhs=cnt[:], start=True, stop=True), reads=["rcnt", "ones"], writes=["rptot"])
            k.op("dve", lambda: nc.vector.tensor_tensor(out=ge[:], in0=ptot[:], in1=kc[:], op=ALU.is_ge), reads=["rptot", "rkc"], writes=["rge"])
            k.op("dve", lambda: nc.vector.scalar_tensor_tensor(out=lo[:], in0=ge[:], scalar=step, in1=lo[:], op0=ALU.mult, op1=ALU.add), reads=["rge", "rlo"], writes=["rlo"])
        masks(lo, "rlo")
        k.op("dve", lambda: nc.vector.tensor_tensor_scan(out=csl[:].rearrange("p e f -> p (e f)"), data0=rml[:].rearrange("p e f -> p (e f)"),
                                                         data1=cml[:].rearrange("p e f -> p (e f)"), initial=0.0, op0=ALU.mult, op1=ALU.add),
             reads=["rcml", "rrm"], writes=["rcsl"])
        k.op("dve", lambda: nc.vector.tensor_tensor_scan(out=csc[:].rearrange("p e f -> p (e f)"), data0=rmc[:].rearrange("p e f -> p (e f)"),
                                                         data1=cmc[:].rearrange("p e f -> p (e f)"), initial=0.0, op0=ALU.mult, op1=ALU.add),
             reads=["rcmc", "rrm"], writes=["rcsc"])
        k.op("dve", lambda: nc.vector.tensor_copy(out=tot[:, 0:16], in_=csl[:, :, 63]), reads=["rcsl"], writes=["rtot"])
        k.op("dve", lambda: nc.vector.tensor_copy(out=tot[:, 16:32], in_=csc[:, :, 1]), reads=["rcsc"], writes=["rtot"])
        k.op("pe", lambda: nc.tensor.matmul(poff[:], lhsT=lstr[:], rhs=tot[:], start=True, stop=True), reads=["rtot", "rlstr"], writes=["rpoff"])
        k.op("dve", lambda: nc.vector.tensor_copy(out=off[:], in_=poff[:]), reads=["rpoff"], writes=["roff"])
        for (cs_, cm_, g_, s_, gi_, si_, o0, F_, cap, base) in ((csl, cml, gl, sl, gli, sli, 0, 64, CAPL, 0), (csc, cmc, gc, sc, gci, sci, 16, 2, CAPC, CAPL)):
            k.op("dve", lambda: nc.vector.tensor_tensor(out=cs_[:], in0=cs_[:], in1=off[:, o0:o0 + 16].unsqueeze(2).to_broadcast([128, NE, F_]), op=ALU.add),
                 reads=["rcsl", "rcsc", "roff"], writes=["rcsl", "rcsc"])
            k.op("dve", lambda: nc.vector.tensor_scalar(out=g_[:], in0=cs_[:], scalar1=float(cap), scalar2=None, op0=ALU.is_le), reads=["rcsl", "rcsc"], writes=["rg"])
            k.op("dve", lambda: nc.vector.tensor_tensor(out=cm_[:], in0=cm_[:], in1=g_[:], op=ALU.mult), reads=["rg", "rcml", "rcmc"], writes=["rcml", "rcmc"])
            k.op("dve", lambda: nc.vector.tensor_scalar(out=g_[:], in0=cs_[:], scalar1=float(base - 1 - SLOTS), scalar2=None, op0=ALU.add), reads=["rcsl", "rcsc"], writes=["rg"])
            k.op("dve", lambda: nc.vector.tensor_tensor(out=g_[:], in0=g_[:], in1=cm_[:], op=ALU.mult), reads=["rg", "rcml", "rcmc"], writes=["rg"])
            k.op("dve", lambda: nc.vector.tensor_tensor(out=g_[:], in0=g_[:], in1=g.ebase[:].unsqueeze(2).to_broadcast([128, NE, F_]), op=ALU.add), reads=["rg", "ebase"], writes=["rg"])
            k.op("dve", lambda: nc.vector.tensor_scalar(out=s_[:], in0=cm_[:], scalar1=-BIGI, scalar2=BIGI, op0=ALU.mult, op1=ALU.add), reads=["rcml", "rcmc"], writes=["rs"])
            k.op("dve", lambda: nc.vector.tensor_tensor(out=s_[:], in0=s_[:], in1=g_[:], op=ALU.add), reads=["rs", "rg"], writes=["rs"])
            k.op("dve", lambda: nc.vector.tensor_copy(out=gi_[:], in_=g_[:]), reads=["rg"], writes=["rgi"])
            k.op("dve", lambda: nc.vector.tensor_copy(out=si_[:], in_=s_[:]), reads=["rs"], writes=["rsi"])
        k.dma("sp", g.gix[:, TC:].rearrange("e (p f) -> p e f", f=64), gli[:], reads=["rgi"], writes=[("dram", "gix")])
        k.dma("act", g.gix[:, 0:TC].rearrange("e (p f) -> p e f", f=2), gci[:], reads=["rgi"], writes=[("dram", "gix")])
        breg = nc.gpsimd.to_reg(NE * SROWS - 1)
        hi = 0
        for (F_, si_, rbase) in ((64, sli, TC), (2, sci, 0)):
            for f in range(F_):
                h_ = ht[hi % 3]
                kh = ("rht", hi % 3)
                hi += 1
                k.dma(None, h_[:], g.h2tok[rbase:rbase + 128 * F_, :].rearrange("(p f) d -> p f d", f=F_)[:, f, :], reads=[("dram", "h2tok")], writes=[kh])
                for e in range(NE):
                    k.dma("pool", g.xsd, h_[:], reads=[kh, "rsi"], writes=[("dram", "xsd")],
                          indirect=dict(out_offset=bass.IndirectOffsetOnAxis(ap=si_[:, e, f:f + 1], axis=0), in_offset=None,
                                        bounds_check=breg, oob_is_err=False))


SBLK = [(0, 512), (512, 512), (1024, 32)]


def phase_experts(g, l, experts=range(NE)):
    k, nc = g.k, g.nc
    NT = (SLOTS + 127) // 128
    with ExitStack() as st:
        wg = k.sb(st, "exwg", [128, KD, DFF], BF16)
        wu = k.sb(st, "exwu", [128, KD, DFF], BF16)
        wd = k.sb(st, "exwd", [128, 8, D], BF16)
        stg = [k.sb(st, "exstg%d" % i, [128, D], F32) for i in range(2)]
        xt = [k.sb(st, "exxt%d" % i, [128, D], BF16) for i in range(2)]
        xsT = k.sb(st, "exxsT", [128, KD, SLOTS], BF16)
        hid = k.sb(st, "exhid", [128, 8, SLOTS], BF16)
        sgt = [k.sb(st, "exsg%d" % i, [128, 512], BF16) for i in range(2)]
        yt = [k.sb(st, "exyt%d" % i, [128, D], BF16) for i in range(2)]
        ptx = [k.ps(st, "exptx%d" % i, [128, 4, 128], BF16) for i in range(2)]
        pg = [k.ps(st, "expg%d" % i, [128, 512]) for i in range(2)]
        pu = [k.ps(st, "expu%d" % i, [128, 512]) for i in range(2)]
        pd = [k.ps(st, "expd%d" % i, [128, 512]) for i in range(2)]
        ci = 0
        xi = 0
        pi = 0
        qi = 0
        yi = 0
        def cast(dst, src, rk, wk):
            k.op("pool", lambda: nc.gpsimd.tensor_copy(out=dst, in_=src), reads=[rk], writes=[wk])

        def load_gu(e):
            nonlocal ci
            for (wsb, wsrc, nk, wk) in ((wg, g.w_eg, KD, "exwg"), (wu, g.w_eu, KD, "exwu")):
                for k2 in range(0, nk, 2):
                    sg = stg[ci % 2]
                    ks = ("exstg", ci % 2)
                    k.dma("sp", sg[:].rearrange("p (a c) -> p a c", a=2), wsrc[l, e][k2 * 128:(k2 + 2) * 128, :].rearrange("(a p) c -> p a c", p=128), writes=[ks])
                    cast(wsb[:, k2:k2 + 2, :], sg[:].rearrange("p (a c) -> p a c", a=2), ks, wk)
                    ci += 1

        def load_d(e):
            nonlocal ci
            for k2 in range(8):
                sg = stg[ci % 2]
                ks = ("exstg", ci % 2)
                k.dma("sp", sg[:], g.w_ed[l, e][k2 * 128:(k2 + 1) * 128, :], writes=[ks])
                cast(wd[:, k2, :], sg[:], ks, "exwd")
                ci += 1

        experts = list(experts)
        load_gu(experts[0])
        load_d(experts[0])
        for ei, e in enumerate(experts):
            nxt = experts[ei + 1] if ei + 1 < len(experts) else None
            for j in range(NT):
                rows = min(128, SLOTS - j * 128)
                x_ = xt[xi % 2]
                kx = ("exxt", xi % 2)
                xi += 1
                r0 = e * SROWS + j * 128
                k.dma("act", x_[:rows, :], g.xsd[r0:r0 + rows, :], reads=[("dram", "xsd")], writes=[kx])
                for k4 in range(0, KD, 4):
                    p_ = ptx[pi % 2]
                    kp = ("exptx", pi % 2)
                    pi += 1
                    for a in range(4):
                        kk = k4 + a
                        k.op("pe", lambda: nc.tensor.transpose(out=p_[:, a, :rows], in_=x_[:rows, kk * 128:(kk + 1) * 128], identity=g.ident_bf[:rows, :rows]),
                             reads=[kx, "ident"], writes=[kp])
                    k.op("dve" if (pi % 2) else "act",
                         (lambda: nc.vector.tensor_copy(out=xsT[:, k4:k4 + 4, j * 128:j * 128 + rows], in_=p_[:, :, :rows])) if (pi % 2) else
                         (lambda: nc.scalar.copy(out=xsT[:, k4:k4 + 4, j * 128:j * 128 + rows], in_=p_[:, :, :rows])),
                         reads=[kp], writes=["exxsT"])
            for fc in range(8):
                for (s0, sw) in SBLK:
                    g_, u_ = pg[qi % 2], pu[qi % 2]
                    kg_, ku_ = ("expg", qi % 2), ("expu", qi % 2)
                    sg_ = sgt[qi % 2]
                    ksg = ("exsg", qi % 2)
                    qi += 1
                    for kk in range(KD):
                        k.op("pe", lambda: nc.tensor.matmul(g_[:, :sw], lhsT=wg[:, kk, fc * 128:(fc + 1) * 128], rhs=xsT[:, kk, s0:s0 + sw], start=(kk == 0), stop=(kk == KD - 1)),
                             reads=["exwg", "exxsT"], writes=[kg_])
                    for kk in range(KD):
                        k.op("pe", lambda: nc.tensor.matmul(u_[:, :sw], lhsT=wu[:, kk, fc * 128:(fc + 1) * 128], rhs=xsT[:, kk, s0:s0 + sw], start=(kk == 0), stop=(kk == KD - 1)),
                             reads=["exwu", "exxsT"], writes=[ku_])
                    k.op("act", lambda: nc.scalar.activation(out=sg_[:, :sw], in_=g_[:, :sw], func=AF.Silu), reads=[kg_], writes=[ksg])
                    k.op("dve", lambda: nc.vector.tensor_tensor(out=hid[:, fc, s0:s0 + sw], in0=u_[:, :sw], in1=sg_[:, :sw], op=ALU.mult), reads=[ku_, ksg], writes=["exhid"])
            if nxt is not None:
                load_gu(nxt)
            for j in range(NT):
                rows = min(128, SLOTS - j * 128)
                y_ = yt[yi % 2]
                ky = ("exyt", yi % 2)
                yi += 1
                for db in range(4):
                    d_ = pd[(yi * 4 + db) % 2]
                    kd_ = ("expd", (yi * 4 + db) % 2)
                    for fc in range(8):
                        k.op("pe", lambda: nc.tensor.matmul(d_[:rows, :], lhsT=hid[:, fc, j * 128:j * 128 + rows], rhs=wd[:, fc, db * 512:(db + 1) * 512], start=(fc == 0), stop=(fc == 7)),
                             reads=["exhid", "exwd"], writes=[kd_])
                    if db % 2 == 0:
                        k.op("act", lambda: nc.scalar.copy(out=y_[:rows, db * 512:(db + 1) * 512], in_=d_[:rows, :]), reads=[kd_], writes=[ky])
                    else:
                        k.op("dve", lambda: nc.vector.tensor_copy(out=y_[:rows, db * 512:(db + 1) * 512], in_=d_[:rows, :]), reads=[kd_], writes=[ky])
                r0 = e * SROWS + j * 128
                k.dma("act", g.ysd[r0:r0 + rows, :], y_[:rows, :], reads=[ky], writes=[("dram", "ysd")])
            if nxt is not None:
                load_d(nxt)


def phase_combine(g, l):
    k, nc = g.k, g.nc
    with ExitStack() as st:
        gi = [k.sb(st, "cbgi%d" % i, [128, NE], I32) for i in range(2)]
        aw = [k.sb(st, "cbaw%d" % i, [128, NE], F32) for i in range(2)]
        dg = [k.sb(st, "cbdg%d" % i, [128, NE, 128], BF16) for i in range(2)]
        G = [k.sb(st, "cbG%d" % i, [128, D], BF16) for i in range(NE)]
        xb = [k.sb(st, "cbx%d" % i, [128, KD, 128], F32) for i in range(2)]
        pacc = [k.ps(st, "cbacc%d" % i, [128, 4, 128]) for i in range(4)]
        gq = 0
        for ti in range(T // 128):
            c0 = ti * 128
            s = 1 if c0 < TC else 0
            gi_, aw_, dg_, x_ = gi[ti % 2], aw[ti % 2], dg[ti % 2], xb[ti % 2]
            kgi, kaw, kdg, kx = ("cbgi", ti % 2), ("cbaw", ti % 2), ("cbdg", ti % 2), ("cbx", ti % 2)
            k.dma("sp", gi_[:], g.gix[:, c0:c0 + 128].rearrange("e t -> t e"), reads=[("dram", "gix")], writes=[kgi], allow_slow_non_contiguous=True)
            k.dma("act", aw_[:], g.affd[:, c0:c0 + 128].rearrange("e t -> t e"), reads=[("dram", "affd")], writes=[kaw], allow_slow_non_contiguous=True)
            k.dma(None, x_[:], fm(g.xT, c0, 128), reads=[("dram", "xT")], writes=[kx])
            for e in range(NE):
                k.op("dve", lambda: nc.vector.tensor_scalar(out=dg_[:, e, :], in0=g.ident_bf[:], scalar1=aw_[:, e:e + 1], scalar2=None, op0=ALU.mult),
                     reads=["ident", kaw], writes=[kdg])
            for e in range(NE):
                k.dma("pool", G[e][:], g.ysd, reads=[kgi, ("dram", "ysd")], writes=[("cbG", e)],
                      indirect=dict(out_offset=None, in_offset=bass.IndirectOffsetOnAxis(ap=gi_[:, e:e + 1], axis=0)))
            for kk in range(KD):
                for e in range(NE):
                    k.op("pe", lambda: nc.tensor.matmul(pacc[kk // 4][:, kk % 4, :], lhsT=G[e][:, kk * 128:(kk + 1) * 128], rhs=dg_[:, e, :], start=(e == 0), stop=(e == NE - 1)),
                         reads=[("cbG", e), kdg], writes=["cbacc"])
            for kk in range(KD):
                k.op("dve", lambda: nc.vector.scalar_tensor_tensor(out=x_[:, kk, :], in0=pacc[kk // 4][:, kk % 4, :], scalar=g.mods[:, l, 5, kk, s:s + 1], in1=x_[:, kk, :],
                                                                   op0=ALU.mult, op1=ALU.add), reads=["cbacc", kx, "mods"], writes=[kx])
            k.dma(None, fm(g.xT, c0, 128), x_[:], reads=[kx], writes=[("dram", "xT")])


_PROG = None


def kernel(**inputs):
    global _PROG
    if _PROG is None:
        _PROG = make_program()
    nc, _ = _PROG
    m = host_inputs(inputs)
    res = run_bass_kernel_spmd(nc, [m], core_ids=[0])
    outT = np.asarray(res.results[0]["outT"], np.float32)
    return np.ascontiguousarray(outT.T)[None]
```
